# Optimizing a Trainium2 kernel written in Bass

```python
import math
import jax, jax.numpy as jnp
from jax import lax
import numpy as np

D_MODEL = 2048
BATCH = 16
SEQ = 2048
DEPTH = 2

MEM_LEN = 256
RET_HEADS = 4
RET_WIDTH = D_MODEL // 4
RET_DV = RET_WIDTH // RET_HEADS
RET_DK = RET_DV
RET_CHUNK = 128
S5_WIDTH = D_MODEL // 4
S5_GROUP = 16
S5_GROUPS = S5_WIDTH // S5_GROUP
S5_STATE = 64
DIFF_WIDTH = D_MODEL // 2
DIFF_HEADS = 8
DIFF_DV = DIFF_WIDTH // DIFF_HEADS
DIFF_DQK = DIFF_DV // 2
Q_BLOCK = 128
REL_BUCKETS = 32
REL_MAX_DIST = 128
X_HEADS = 4
X_HEAD_DIM = 128
X_WIDTH = X_HEADS * X_HEAD_DIM
D_FF = 4 * D_MODEL
EPS = 1e-6

MIX_WIDTH = RET_WIDTH + S5_WIDTH + DIFF_WIDTH
RET_QK_W = RET_HEADS * RET_DK
DIFF_QK_W = DIFF_HEADS * 2 * DIFF_DQK
IN_SIZES = (RET_QK_W, RET_QK_W, RET_WIDTH, RET_WIDTH, S5_WIDTH, DIFF_QK_W, DIFF_QK_W, DIFF_WIDTH)
IN_WIDTH = int(sum(IN_SIZES))
IN_SPLITS = tuple(int(v) for v in np.cumsum(IN_SIZES)[:-1])

kernel_name = 'hybrid_ret_s5_diffattn_block'


def rmsnorm(x, g):
    xf = x.astype(jnp.float32)
    y = xf * lax.rsqrt(jnp.mean(xf * xf, axis=-1, keepdims=True) + EPS)
    return (y * g.astype(jnp.float32)).astype(x.dtype)


def rotate(x, pos):
    half = x.shape[-1] // 2
    inv = 10000.0 ** (-jnp.arange(half, dtype=jnp.float32) / half)
    ang = pos.astype(jnp.float32)[..., None] * inv
    cos = jnp.cos(ang)[:, :, None, :]
    sin = jnp.sin(ang)[:, :, None, :]
    xf = x.astype(jnp.float32)
    x1, x2 = xf[..., :half], xf[..., half:]
    return jnp.concatenate([x1 * cos - x2 * sin, x1 * sin + x2 * cos], axis=-1)


def retention_group(q, k, v, gate, pos):
    B, S = q.shape[:2]
    H, C = RET_HEADS, RET_CHUNK
    n = S // C
    q = rotate(q.reshape(B, S, H, RET_DK), pos)
    k = rotate(k.reshape(B, S, H, RET_DK), pos) * (RET_DK ** -0.5)
    v = v.reshape(B, S, H, RET_DV).astype(jnp.float32)

    def chunks(t):
        return jnp.moveaxis(t.reshape(B, n, C, H, t.shape[-1]), 1, 0)

    log_g = jnp.log(1.0 - 2.0 ** (-5.0 - jnp.arange(H, dtype=jnp.float32)))
    idx = jnp.arange(C, dtype=jnp.float32)
    dist = idx[:, None] - idx[None, :]
    decay = jnp.where(dist >= 0, jnp.exp(jnp.maximum(dist, 0.0)[None] * log_g[:, None, None]), 0.0)
    xi = jnp.exp((idx + 1.0)[:, None] * log_g[None, :])
    zeta = jnp.exp((C - 1.0 - idx)[:, None] * log_g[None, :])
    g_chunk = jnp.exp(C * log_g)

    def step(R, qkv):
        qc, kc, vc = qkv
        inner = jnp.einsum('bchd,bshd->bhcs', qc, kc) * decay
        o = jnp.einsum('bhcs,bshe->bche', inner, vc)
        o = o + jnp.einsum('bchd,bhde->bche', qc * xi[None, :, :, None], R)
        R = g_chunk[None, :, None, None] * R + jnp.einsum('bshd,bshe->bhde', kc * zeta[None, :, :, None], vc)
        return R, o

    R0 = jnp.zeros((B, H, RET_DK, RET_DV), jnp.float32)
    _, o = lax.scan(step, R0, (chunks(q), chunks(k), chunks(v)))
    o = jnp.moveaxis(o, 0, 1).reshape(B, S, H, RET_DV)
    o = o * lax.rsqrt(jnp.mean(o * o, axis=-1, keepdims=True) + EPS)
    return o.reshape(B, S, RET_WIDTH) * jax.nn.silu(gate.astype(jnp.float32))


def _ssm_combine(e1, e2):
    a1r, a1i, b1r, b1i = e1
    a2r, a2i, b2r, b2i = e2
    ar = a1r * a2r - a1i * a2i
    ai = a1r * a2i + a1i * a2r
    br = a2r * b1r - a2i * b1i + b2r
    bi = a2r * b1i + a2i * b1r + b2i
    return (ar, ai, br, bi)


def s5_group(u, lam_re, lam_im, log_dt, b_re, b_im, c_re, c_im, d_skip, w_glu):
    B, S = u.shape[:2]
    G, P = S5_GROUPS, S5_STATE
    uf = u.astype(jnp.float32).reshape(B, S, G, S5_GROUP)
    lr = jnp.minimum(lam_re.astype(jnp.float32), -1e-4)
    li = lam_im.astype(jnp.float32)
    dt = jnp.exp(log_dt.astype(jnp.float32))[:, None]
    mag = jnp.exp(lr * dt)
    ab_re = mag * jnp.cos(li * dt)
    ab_im = mag * jnp.sin(li * dt)
    nr, ni = ab_re - 1.0, ab_im
    den = lr * lr + li * li
    f_re = ((nr * lr + ni * li) / den)[..., None]
    f_im = ((ni * lr - nr * li) / den)[..., None]
    br, bi = b_re.astype(jnp.float32), b_im.astype(jnp.float32)
    bb_re = f_re * br - f_im * bi
    bb_im = f_re * bi + f_im * br
    bu_re = jnp.einsum('bsgh,gph->sbgp', uf, bb_re)
    bu_im = jnp.einsum('bsgh,gph->sbgp', uf, bb_im)
    a_re = jnp.broadcast_to(ab_re, (S, 1, G, P))
    a_im = jnp.broadcast_to(ab_im, (S, 1, G, P))
    _, _, x_re, x_im = lax.associative_scan(_ssm_combine, (a_re, a_im, bu_re, bu_im), axis=0)
    y = (jnp.einsum('sbgp,ghp->bsgh', x_re, c_re.astype(jnp.float32))
         - jnp.einsum('sbgp,ghp->bsgh', x_im, c_im.astype(jnp.float32)))
    y = y.reshape(B, S, S5_WIDTH) + d_skip.astype(jnp.float32) * uf.reshape(B, S, S5_WIDTH)
    g = jax.nn.gelu(y)
    return g * jax.nn.sigmoid(g @ w_glu.astype(jnp.float32))


def t5_bias(qpos, kpos, table):
    n = jnp.maximum(qpos[:, :, None] - kpos[:, None, :], 0)
    max_exact = REL_BUCKETS // 2
    large = max_exact + (jnp.log(jnp.maximum(n, 1).astype(jnp.float32) / max_exact)
                         / math.log(REL_MAX_DIST / max_exact) * (REL_BUCKETS - max_exact)).astype(jnp.int32)
    large = jnp.minimum(large, REL_BUCKETS - 1)
    bucket = jnp.where(n < max_exact, n, large)
    return jnp.moveaxis(table.astype(jnp.float32)[bucket], -1, 1)


def _block_probs(qa, ka, bias, causal, scale):
    s = jnp.einsum('bqhd,bkhd->bhqk', qa, ka).astype(jnp.float32) * scale + bias
    return jax.nn.softmax(jnp.where(causal, s, -jnp.inf), axis=-1)


def diff_group(q, k, v, pos, rel_bias, lam, lam_init, g_subln):
    B, S = q.shape[:2]
    H = DIFF_HEADS
    q = q.reshape(B, S, H, 2, DIFF_DQK)
    k = k.reshape(B, S, H, 2, DIFF_DQK)
    v = v.reshape(B, S, H, DIFF_DV).astype(jnp.float32)
    q1, q2 = q[..., 0, :], q[..., 1, :]
    k1, k2 = k[..., 0, :], k[..., 1, :]
    scale = DIFF_DQK ** -0.5
    outs = []
    for i in range(S // Q_BLOCK):
        q0 = i * Q_BLOCK
        kend = q0 + Q_BLOCK
        bias = t5_bias(pos[:, q0:kend], pos[:, :kend], rel_bias)
        causal = jnp.arange(kend)[None, :] <= (q0 + jnp.arange(Q_BLOCK))[:, None]
        p1 = _block_probs(q1[:, q0:kend], k1[:, :kend], bias, causal, scale)
        p2 = _block_probs(q2[:, q0:kend], k2[:, :kend], bias, causal, scale)
        outs.append(jnp.einsum('bhqk,bkhe->bqhe', p1 - lam * p2, v[:, :kend]))
    o = jnp.concatenate(outs, axis=1)
    o = rmsnorm(o, g_subln) * (1.0 - lam_init)
    return o.reshape(B, S, DIFF_WIDTH)


def cross_attention(h, mem_n, w_q, w_kv, w_o):
    B, S = h.shape[:2]
    M = mem_n.shape[1]
    q = (h @ w_q).reshape(B, S, X_HEADS, X_HEAD_DIM)
    k, v = jnp.split(mem_n @ w_kv, 2, axis=-1)
    k = k.reshape(B, M, X_HEADS, X_HEAD_DIM)
    v = v.reshape(B, M, X_HEADS, X_HEAD_DIM)
    s = jnp.einsum('bshd,bmhd->bhsm', q, k).astype(jnp.float32) * (X_HEAD_DIM ** -0.5)
    p = jax.nn.softmax(s, axis=-1).astype(v.dtype)
    o = jnp.einsum('bhsm,bmhd->bshd', p, v).reshape(B, S, X_WIDTH)
    return o @ w_o


def setup_inputs(seed: int = 0) -> dict:
    key = jax.random.key(seed)
    ks = jax.random.split(key, 32)
    f32 = jnp.float32
    nrm = lambda k, shape, s: jax.random.normal(k, shape, f32) * s
    gain = lambda k, shape: 1.0 + 0.02 * jax.random.normal(k, shape, f32)
    offs = jax.random.randint(ks[2], (BATCH, 1), 0, 4096, dtype=jnp.int32)
    positions = offs + jnp.arange(SEQ, dtype=jnp.int32)[None, :]
    lam_im0 = math.pi * jnp.arange(S5_STATE, dtype=f32)
    return {
        'x': nrm(ks[0], (BATCH, SEQ, D_MODEL), 1.0),
        'mem': nrm(ks[1], (BATCH, MEM_LEN, D_MODEL), 1.0),
        'positions': positions,
        'rel_bias': nrm(ks[3], (REL_BUCKETS, DIFF_HEADS), 0.5),
        'w_in': nrm(ks[4], (DEPTH, D_MODEL, IN_WIDTH), D_MODEL ** -0.5),
        'w_out': nrm(ks[5], (DEPTH, MIX_WIDTH, D_MODEL), MIX_WIDTH ** -0.5),
        'lam_re': -0.5 + nrm(ks[6], (DEPTH, S5_GROUPS, S5_STATE), 0.01),
        'lam_im': lam_im0 + nrm(ks[7], (DEPTH, S5_GROUPS, S5_STATE), 0.01),
        'log_dt': jax.random.uniform(ks[8], (DEPTH, S5_GROUPS), f32, math.log(1e-3), math.log(1e-1)),
        'b_re': nrm(ks[9], (DEPTH, S5_GROUPS, S5_STATE, S5_GROUP), (2 * S5_GROUP) ** -0.5),
        'b_im': nrm(ks[10], (DEPTH, S5_GROUPS, S5_STATE, S5_GROUP), (2 * S5_GROUP) ** -0.5),
        'c_re': nrm(ks[11], (DEPTH, S5_GROUPS, S5_GROUP, S5_STATE), S5_STATE ** -0.5),
        'c_im': nrm(ks[12], (DEPTH, S5_GROUPS, S5_GROUP, S5_STATE), S5_STATE ** -0.5),
        'd_skip': nrm(ks[13], (DEPTH, S5_WIDTH), 1.0),
        'w_glu': nrm(ks[14], (DEPTH, S5_WIDTH, S5_WIDTH), S5_WIDTH ** -0.5),
        'lam_q1': nrm(ks[15], (DEPTH, DIFF_DQK), 0.1),
        'lam_k1': nrm(ks[16], (DEPTH, DIFF_DQK), 0.1),
        'lam_q2': nrm(ks[17], (DEPTH, DIFF_DQK), 0.1),
        'lam_k2': nrm(ks[18], (DEPTH, DIFF_DQK), 0.1),
        'g_subln': gain(ks[19], (DEPTH, DIFF_DV)),
        'w_xq': nrm(ks[20], (DEPTH, D_MODEL, X_WIDTH), D_MODEL ** -0.5),
        'w_xkv': nrm(ks[21], (DEPTH, D_MODEL, 2 * X_WIDTH), D_MODEL ** -0.5),
        'w_xo': nrm(ks[22], (DEPTH, X_WIDTH, D_MODEL), X_WIDTH ** -0.5),
        'w_up': nrm(ks[23], (DEPTH, D_MODEL, D_FF), D_MODEL ** -0.5),
        'w_down': nrm(ks[24], (DEPTH, D_FF, D_MODEL), D_FF ** -0.5),
        'g_mix_pre': gain(ks[25], (DEPTH, D_MODEL)),
        'g_mix_post': gain(ks[26], (DEPTH, D_MODEL)),
        'g_mem': gain(ks[27], (DEPTH, D_MODEL)),
        'g_x_pre': gain(ks[28], (DEPTH, D_MODEL)),
        'g_x_post': gain(ks[29], (DEPTH, D_MODEL)),
        'g_mlp_pre': gain(ks[30], (DEPTH, D_MODEL)),
        'g_mlp_post': gain(ks[31], (DEPTH, D_MODEL)),
    }


def reference(x, mem, positions, rel_bias, w_in, w_out, lam_re, lam_im, log_dt, b_re, b_im,
              c_re, c_im, d_skip, w_glu, lam_q1, lam_k1, lam_q2, lam_k2, g_subln,
              w_xq, w_xkv, w_xo, w_up, w_down, g_mix_pre, g_mix_post, g_mem,
              g_x_pre, g_x_post, g_mlp_pre, g_mlp_post):
    for l in range(DEPTH):
        lam_init = 0.8 - 0.6 * math.exp(-0.3 * l)
        h = rmsnorm(x, g_mix_pre[l])
        proj = h @ w_in[l]
        rq, rk, rv, rg, su, dq, dk, dv = jnp.split(proj, IN_SPLITS, axis=-1)
        y_ret = retention_group(rq, rk, rv, rg, positions)
        y_s5 = s5_group(su, lam_re[l], lam_im[l], log_dt[l], b_re[l], b_im[l],
                        c_re[l], c_im[l], d_skip[l], w_glu[l])
        lam = (jnp.exp(jnp.sum(lam_q1[l].astype(jnp.float32) * lam_k1[l].astype(jnp.float32)))
               - jnp.exp(jnp.sum(lam_q2[l].astype(jnp.float32) * lam_k2[l].astype(jnp.float32)))
               + lam_init)
        y_diff = diff_group(dq, dk, dv, positions, rel_bias, lam, lam_init, g_subln[l])
        mixed = jnp.concatenate([y_ret, y_s5, y_diff], axis=-1).astype(x.dtype) @ w_out[l]
        x = x + rmsnorm(mixed, g_mix_post[l])
        h = rmsnorm(x, g_x_pre[l])
        mem_n = rmsnorm(mem, g_mem[l])
        x = x + rmsnorm(cross_attention(h, mem_n, w_xq[l], w_xkv[l], w_xo[l]), g_x_post[l])
        h = rmsnorm(x, g_mlp_pre[l])
        y = jnp.square(jax.nn.relu(h @ w_up[l])) @ w_down[l]
        x = x + rmsnorm(y, g_mlp_post[l])
    return x
```

```python
import math
from contextlib import ExitStack

import numpy as np
import ml_dtypes

import concourse.bass as bass
import concourse.mybir as mybir
from concourse.bass_utils import run_bass_kernel_spmd

F32 = mybir.dt.float32
BF16 = mybir.dt.bfloat16
I32 = mybir.dt.int32
ALU = mybir.AluOpType
AF = mybir.ActivationFunctionType
AX = mybir.AxisListType

D = 2048
DC = 16
DFF = 8192
INW = 5632
EPS = 1e-6
TWO_PI = 2.0 * math.pi


class Cfg:
    def __init__(self, S=2048, NSEQ=2, DEPTH=2, MEM=256, debug=False, phases=None):
        self.S, self.NSEQ, self.DEPTH, self.MEM, self.debug = S, NSEQ, DEPTH, MEM, debug
        self.NT = S // 128
        self.NG = S // 512
        self.phases = phases
        import os
        self.bcut = int(os.environ.get('BCUT', 9))
        self.gcut = int(os.environ.get('GCUT', 9))


SMALL = {"cvec", "dsc", "s5s", "s5cr", "s5dsk", "tb", "rc_zeta", "rc_gch"}
SMALL_PFX = ("s5ini", "ssqG")
SMALL_SFX = ("_ss0", "_ss1", "_tss0", "_tss1")


class Sch:
    def __init__(self, nc, st):
        self.nc = nc
        self.E = dict(pe=nc.tensor, act=nc.scalar, dve=nc.vector, pool=nc.gpsimd, sp=nc.sync)
        self.sem = {k: st.enter_context(nc.semaphore("e_" + k)) for k in self.E}
        self.cnt = {k: 0 for k in self.E}
        self.pool_sems = [st.enter_context(nc.semaphore("d%d" % i)) for i in range(88)]
        self.pool_cnt = [0] * len(self.pool_sems)
        self.free = list(range(len(self.pool_sems)))
        self.kmap = {}
        self.persist = set()
        self.seen = {k: {} for k in self.E}
        self.lastw = {}
        self.readers = {}

    def _semof(self, key):
        return self.sem[key[1]] if key[0] == "E" else self.pool_sems[key[1]]

    def _wait(self, eng, toks):
        for key, val, small in toks:
            if key == ("E", "pe") and eng == "pe":
                continue
            if self.seen[eng].get(key, 0) >= val:
                continue
            self.E[eng].wait_ge(self._semof(key), val)
            self.seen[eng][key] = val

    def is_small(self, x):
        return x in SMALL or any(x.startswith(p) for p in SMALL_PFX) or x.endswith(SMALL_SFX)

    def _deps(self, r, w):
        toks = []
        for x in r:
            sm = self.is_small(x)
            if x in self.lastw:
                toks.append(self.lastw[x] + (sm,))
        for x in w:
            sm = self.is_small(x)
            if x in self.lastw:
                toks.append(self.lastw[x] + (sm,))
            toks.extend((k, v, sm) for k, v in self.readers.get(x, {}).items())
        return toks

    def _record(self, tok, r, w):
        for x in r:
            d = self.readers.setdefault(x, {})
            d[tok[0]] = max(d.get(tok[0], 0), tok[1])
        for x in w:
            self.lastw[x] = tok
            self.readers[x] = {}

    def op(self, eng, fn, r=(), w=()):
        self._wait(eng, self._deps(r, w))
        ins = fn(self.E[eng])
        self.cnt[eng] += 1
        ins.then_inc(self.sem[eng], 1)
        self._record((("E", eng), self.cnt[eng]), r, w)

    def dkey(self, name, persist=False):
        if name not in self.kmap:
            self.kmap[name] = self.free.pop(0)
            if persist:
                self.persist.add(name)
        return self.kmap[name]

    def dma(self, q, out, in_, key, r=(), w=(), persist=False, **kw):
        self._wait(q, self._deps(r, w))
        ki = self.dkey(key, persist)
        ins = self.E[q].dma_start(out=out, in_=in_, **kw)
        self.pool_cnt[ki] += 16
        ins.then_inc(self.pool_sems[ki], 16)
        self._record((("D", ki), self.pool_cnt[ki]), r, w)

    def barrier(self):
        toks = [(("E", e), self.cnt[e], True) for e in self.E if self.cnt[e] > 0]
        toks += [(("D", i), c, True) for i, c in enumerate(self.pool_cnt) if c > 0]
        for e in self.E:
            self._wait(e, toks)
        self.lastw.clear()
        self.readers.clear()
        for name in list(self.kmap):
            if name not in self.persist:
                self.free.append(self.kmap.pop(name))

    def release_key(self, name):
        self.persist.discard(name)


_UNIQ = [0]


class Ring:
    def __init__(self, nc, st, name, shape, dtype, n, psum=False):
        alloc = nc.psum_tensor if psum else nc.sbuf_tensor
        _UNIQ[0] += 1
        self.t = [st.enter_context(alloc("%s%d_u%d" % (name, i, _UNIQ[0]), shape, dtype)) for i in range(n)]
        self.names = ["%s%d" % (name, i) for i in range(n)]
        self.i = -1

    def next(self):
        self.i = (self.i + 1) % len(self.t)
        return self.t[self.i], self.names[self.i]


class VRing:
    def __init__(self, views, names):
        self.t, self.names, self.i = views, names, -1

    def next(self):
        self.i = (self.i + 1) % len(self.t)
        return self.t[self.i], self.names[self.i]


def t5_bucket(n):
    n = max(n, 0)
    if n < 16:
        return n
    v = 16 + int(np.float32(np.log(np.float32(max(n, 1)) / np.float32(16.0))) / np.float32(math.log(128 / 16)) * np.float32(16))
    return min(v, 31)


def host_consts():
    c = {}
    c["ident"] = np.eye(128, dtype=np.float32).astype(ml_dtypes.bfloat16)
    c["ones"] = np.ones((128, 128), np.float32).astype(ml_dtypes.bfloat16)
    p = np.arange(128)
    cv = np.zeros((128, 8), np.float32)
    cv[:, 0] = 10000.0 ** (-(p % 64) / 64.0)
    cv[:, 1] = np.where(p < 64, -1.0, 1.0)
    cv[:, 2] = (p < 64).astype(np.float32)
    cv[:, 3] = (p >= 64).astype(np.float32)
    cv[:, 4] = EPS
    cv[:, 5] = math.pi / 2
    c["cvec"] = cv
    H, C = 4, 128
    log_g = np.log(1.0 - 2.0 ** (-5.0 - np.arange(H, dtype=np.float64)))
    idx = np.arange(C, dtype=np.float64)
    sc = 128 ** -0.5
    dec = np.zeros((128, H, 128), np.float32)
    for h in range(H):
        dist = idx[None, :] - idx[:, None]
        dec[:, h, :] = np.where(dist >= 0, np.exp(np.maximum(dist, 0) * log_g[h]), 0.0) * sc
    c["decay"] = dec
    xi = np.zeros((128, H, 128), np.float32)
    zt = np.zeros((128, H), np.float32)
    gc = np.zeros((128, H), np.float32)
    for h in range(H):
        xi[:, h, :] = np.exp((idx + 1.0) * log_g[h])[None, :]
        zt[:, h] = np.exp((C - 1.0 - idx) * log_g[h]) * sc
        gc[:, h] = np.exp(C * log_g[h])
    c["xi"] = xi
    c["zeta"] = zt
    c["gch"] = gc
    mk = np.zeros((128, 4, 128), np.float32)
    for kk in range(4):
        for g2 in range(2):
            mk[g2 * 64:(g2 + 1) * 64, kk, 32 * kk + 16 * g2: 32 * kk + 16 * g2 + 16] = 1.0
    c["s5mask"] = mk
    c["iota512"] = np.broadcast_to(np.arange(512, dtype=np.float32)[None, :], (128, 512)).copy()
    oh = np.zeros((2, 128, 32, 128), np.float32)
    cm = np.zeros((128, 128), np.float32)
    for s in range(128):
        for t in range(128):
            if t >= s:
                oh[0, s, t5_bucket(t - s), t] = 1.0
            else:
                cm[s, t] = -200.0
            oh[1, s, t5_bucket(128 + t - s), t] = 1.0
    c["oh"] = oh
    c["cmask"] = cm
    return c


CONST_SPECS = [("ident", [128, 128], BF16), ("ones", [128, 128], BF16), ("cvec", [128, 8], F32),
               ("decay", [128, 4, 128], F32), ("xi", [128, 4, 128], F32), ("zeta", [128, 4], F32),
               ("gch", [128, 4], F32), ("s5mask", [128, 4, 128], F32), ("iota512", [128, 512], F32),
               ("oh", [2, 128, 32, 128], F32), ("cmask", [128, 128], F32)]

PARAM_SPECS = [
    ("rel_bias", [32, 8]), ("w_in", [None, D, INW]), ("w_out", [None, D, D]), ("lam_re", [None, 32, 64]),
    ("lam_im", [None, 32, 64]), ("log_dt", [None, 32]), ("b_re", [None, 32, 64, 16]), ("b_im", [None, 32, 64, 16]),
    ("c_re", [None, 32, 16, 64]), ("c_im", [None, 32, 16, 64]), ("d_skip", [None, 512]), ("w_glu", [None, 512, 512]),
    ("lam_q1", [None, 64]), ("lam_k1", [None, 64]), ("lam_q2", [None, 64]), ("lam_k2", [None, 64]),
    ("g_subln", [None, 128]), ("w_xq", [None, D, 512]), ("w_xkv", [None, D, 1024]), ("w_xo", [None, 512, D]),
    ("w_up", [None, D, DFF]), ("w_down", [None, DFF, D]), ("g_mix_pre", [None, D]), ("g_mix_post", [None, D]),
    ("g_mem", [None, D]), ("g_x_pre", [None, D]), ("g_x_post", [None, D]), ("g_mlp_pre", [None, D]),
    ("g_mlp_post", [None, D]),
]


def build(cfg):
    nc = bass.Bass("TRN2", target_bir_lowering=False)
    S_, NT, NG, NSEQ, DEPTH, MEM = cfg.S, cfg.NT, cfg.NG, cfg.NSEQ, cfg.DEPTH, cfg.MEM
    MT = MEM // 128
    A = {}
    A["x"] = nc.dram_tensor("x", [NSEQ, S_, D], F32, kind="ExternalInput").ap()
    A["mem"] = nc.dram_tensor("mem", [NSEQ, MEM, D], F32, kind="ExternalInput").ap()
    A["positions"] = nc.dram_tensor("positions", [NSEQ, S_], I32, kind="ExternalInput").ap()
    for name, shp in PARAM_SPECS:
        shp = [DEPTH if v is None else v for v in shp]
        A[name] = nc.dram_tensor(name, shp, F32, kind="ExternalInput").ap()
    for name, shp, dt in CONST_SPECS:
        A["c_" + name] = nc.dram_tensor("c_" + name, shp, dt, kind="ExternalInput").ap()
    out = nc.dram_tensor("out", [NSEQ, S_, D], F32, kind="ExternalOutput").ap()
    skind = "ExternalOutput" if cfg.debug else "Internal"

    def scratch(name, shape, dt=BF16):
        return nc.dram_tensor(name, shape, dt, kind=skind).ap()

    Wb = {}
    for l in range(DEPTH):
        Wb["in", l] = scratch("wb_in%d" % l, [11, 128, 16, 512])
        Wb["insw", l] = scratch("wb_insw%d" % l, [2, 128, 16, 512])
        Wb["out", l] = scratch("wb_out%d" % l, [4, 128, 16, 512])
        Wb["xq", l] = scratch("wb_xq%d" % l, [128, 16, 512])
        Wb["xkv", l] = scratch("wb_xkv%d" % l, [2, 128, 16, 512])
        Wb["xo", l] = scratch("wb_xo%d" % l, [128, 4, D])
        Wb["up", l] = scratch("wb_up%d" % l, [32, 128, 16, 256])
        Wb["down", l] = scratch("wb_down%d" % l, [DFF, D])
        Wb["glu", l] = scratch("wb_glu%d" % l, [128, 4, 512])
    fmT = scratch("fmT", [32, 128, S_])
    rv = scratch("rv", [S_, 512])
    dv = scratch("dv", [S_, 1024])
    yT = scratch("yT", [16, 128, S_])

    def on(ph):
        return cfg.phases is None or ph in cfg.phases

    with ExitStack() as st:
        S = Sch(nc, st)

        def sb(name, shape, dt=F32, stack=st):
            _UNIQ[0] += 1
            return stack.enter_context(nc.sbuf_tensor("%s_u%d" % (name, _UNIQ[0]), shape, dt))

        def ps(name, shape, dt=F32, stack=st):
            _UNIQ[0] += 1
            return stack.enter_context(nc.psum_tensor("%s_u%d" % (name, _UNIQ[0]), shape, dt))

        ident = sb("ident", [128, 128], BF16)
        ones = sb("ones", [128, 128], BF16)
        cvec = sb("cvec", [128, 8])
        Bm = sb("Bm", [128, 8, 2, 128])
        tb = sb("tb", [128, 256])
        S.dma("sp", ident[:], A["c_ident"], "ident", w=["ident"])
        S.dma("sp", ones[:], A["c_ones"], "ones", w=["ones"])
        S.dma("sp", cvec[:], A["c_cvec"], "cvec", w=["cvec"])
        S.dma("sp", tb[:], A["rel_bias"].rearrange("b h -> (b h)").partition_broadcast(128), "tb", w=["tb"])
        inv_ap, sgn_ap, m0_ap, m1_ap, eps_ap, hpi_ap = (cvec[:, i:i + 1] for i in range(6))

        def prepass(l):
            k = "w_in%d" % l
            for b in range(11):
                S.dma("pool", Wb["in", l][b], A["w_in"][l][:, b * 512:(b + 1) * 512].rearrange("(c p) n -> p c n", p=128),
                      k, w=[k], persist=True)
            k = "w_insw%d" % l
            for b in range(2):
                src = A["w_in"][l][:, b * 512:(b + 1) * 512].rearrange("(c p) (h two j) -> p c h two j", p=128, two=2, j=64)
                dst = Wb["insw", l][b].rearrange("p c (h two j) -> p c h two j", two=2, j=64)
                for c4 in range(16):
                    S.dma("pool", dst[:, c4, :, 0, :], src[:, c4, :, 1, :], k, w=[k], persist=True)
                    S.dma("pool", dst[:, c4, :, 1, :], src[:, c4, :, 0, :], k, w=[k], persist=True)
            k = "w_out%d" % l
            for b in range(4):
                S.dma("pool", Wb["out", l][b], A["w_out"][l][:, b * 512:(b + 1) * 512].rearrange("(c p) n -> p c n", p=128),
                      k, w=[k], persist=True)
            k = "w_x%d" % l
            S.dma("pool", Wb["xq", l], A["w_xq"][l].rearrange("(c p) n -> p c n", p=128), k, w=[k], persist=True)
            for b in range(2):
                S.dma("pool", Wb["xkv", l][b], A["w_xkv"][l][:, b * 512:(b + 1) * 512].rearrange("(c p) n -> p c n", p=128),
                      k, w=[k], persist=True)
            S.dma("pool", Wb["xo", l], A["w_xo"][l].rearrange("(c p) n -> p c n", p=128), k, w=[k], persist=True)
            S.dma("pool", Wb["glu", l], A["w_glu"][l].rearrange("(c p) n -> p c n", p=128), k, w=[k], persist=True)
            k = "w_up%d" % l
            for b in range(32):
                S.dma("pool", Wb["up", l][b], A["w_up"][l][:, b * 256:(b + 1) * 256].rearrange("(c p) n -> p c n", p=128),
                      k, w=[k], persist=True)
            k = "w_down%d" % l
            for b in range(8):
                S.dma("pool", Wb["down", l][b * 1024:(b + 1) * 1024, :], A["w_down"][l][b * 1024:(b + 1) * 1024, :],
                      k, w=[k], persist=True)

        prepass(0)

        def sincos_tmp(stk, pre, shape):
            return (sb(pre + "_kf", shape, F32, stk), sb(pre + "_ki", shape, I32, stk), sb(pre + "_rr", shape, F32, stk), pre)

        def sincos(tmp, ang, out_s, out_c, rw):
            kf, ki, rr, pre = tmp
            nm = [pre + "_tmp"]
            for which, dst in (("s", out_s), ("c", out_c)):
                if dst is None:
                    continue
                if which == "c":
                    S.op("dve", lambda e: e.tensor_scalar(rr[:], ang, math.pi / 2, None, ALU.add), r=rw, w=nm)
                    src = rr[:]
                else:
                    src = ang
                S.op("dve", lambda e: e.tensor_scalar(kf[:], src, 1.0 / TWO_PI, None, ALU.mult), r=rw + nm, w=nm)
                S.op("dve", lambda e: e.tensor_copy(ki[:], kf[:]), r=nm, w=nm)
                S.op("dve", lambda e: e.tensor_copy(kf[:], ki[:]), r=nm, w=nm)
                S.op("dve", lambda e: e.scalar_tensor_tensor(rr[:], kf[:], -TWO_PI, src, ALU.mult, ALU.add), r=rw + nm, w=nm)
                S.op("dve", lambda e: e.tensor_scalar(kf[:], rr[:], math.pi, -TWO_PI, ALU.is_gt, ALU.mult), r=nm, w=nm)
                S.op("dve", lambda e: e.tensor_tensor(rr[:], rr[:], kf[:], ALU.add), r=nm, w=nm)
                S.op("dve", lambda e: e.tensor_scalar(kf[:], rr[:], -math.pi, TWO_PI, ALU.is_lt, ALU.mult), r=nm, w=nm)
                S.op("dve", lambda e: e.tensor_tensor(rr[:], rr[:], kf[:], ALU.add), r=nm, w=nm)
                S.op("act", lambda e: e.activation(dst, rr[:], AF.Sin), r=nm, w=rw + nm)

        def load_gamma(stk, name, vec_ap):
            t = sb(name, [128, D], F32, stk)
            S.dma("sp", t[:], vec_ap.partition_broadcast(128), name, w=[name])
            return t

        class NormT:
            def __init__(self, stk, pre):
                self.xin = Ring(nc, stk, pre + "_x", [128, D], F32, 2)
                self.hb = Ring(nc, stk, pre + "_hb", [128, D], BF16, 2)
                self.ss = Ring(nc, stk, pre + "_ss", [128, 2], F32, 2)
                self.pt = Ring(nc, stk, pre + "_pt", [128, 8, 128], BF16, 2, psum=True)
                self.flip = 0

            def run(self, src_ap, gname, gt, hT, hres, col0):
                xs, xn = self.xin.next()
                hb, hn = self.hb.next()
                ss, sn = self.ss.next()
                S.dma("sp", xs[:], src_ap, xn, w=[xn])
                S.op("act", lambda e: e.activation(hb[:], xs[:], AF.Square, accum_out=ss[:, 0:1]), r=[xn], w=[hn, sn])
                S.op("act", lambda e: e.activation(ss[:, 1:2], ss[:, 0:1], AF.Sqrt, bias=eps_ap, scale=1.0 / D), r=[sn, "cvec"], w=[sn])
                S.op("dve", lambda e: e.reciprocal(ss[:, 1:2], ss[:, 1:2]), r=[sn], w=[sn])
                S.op("dve", lambda e: e.scalar_tensor_tensor(hb[:], xs[:], ss[:, 1:2], gt[:], ALU.mult, ALU.mult),
                     r=[xn, sn, gname], w=[hn])
                for half in range(2):
                    pt, pn = self.pt.next()
                    for c in range(8):
                        cc = half * 8 + c
                        S.op("pe", lambda e: e.transpose(pt[:, c, :], hb[:, cc * 128:(cc + 1) * 128], ident[:]),
                             r=[hn, "ident"], w=[pn])
                    eng = "act" if self.flip else "dve"
                    self.flip ^= 1
                    dst = hT[:, half * 8:half * 8 + 8, col0:col0 + 128]
                    if eng == "act":
                        S.op("act", lambda e: e.copy(dst, pt[:]), r=[pn], w=[hres])
                    else:
                        S.op("dve", lambda e: e.tensor_copy(dst, pt[:]), r=[pn], w=[hres])

        class Tail:
            def __init__(self, stk, pre):
                self.x = Ring(nc, stk, pre + "_tx", [128, D], F32, 2)
                self.t = Ring(nc, stk, pre + "_tt", [128, D], F32, 2)
                self.ss = Ring(nc, stk, pre + "_tss", [128, 8], F32, 2)
                self.junk = sb(pre + "_junk", [128, 512], BF16, stk)
                self.jn = pre + "_junk"

            def run(self, banks, bnames, gname, gt, x_ap, out_ap):
                xs, xn = self.x.next()
                tt, tn = self.t.next()
                ss, sn = self.ss.next()
                S.dma("sp", xs[:], x_ap, xn, w=[xn])
                for cb in range(4):
                    S.op("act", lambda e: e.activation(self.junk[:], banks[cb], AF.Square, accum_out=ss[:, cb:cb + 1]),
                         r=[bnames[cb]], w=[self.jn, sn])
                S.op("dve", lambda e: e.tensor_reduce(ss[:, 4:5], ss[:, 0:4], AX.X, ALU.add), r=[sn], w=[sn])
                S.op("act", lambda e: e.activation(ss[:, 5:6], ss[:, 4:5], AF.Sqrt, bias=eps_ap, scale=1.0 / D), r=[sn, "cvec"], w=[sn])
                S.op("dve", lambda e: e.reciprocal(ss[:, 5:6], ss[:, 5:6]), r=[sn], w=[sn])
                for cb in range(4):
                    cs = slice(cb * 512, (cb + 1) * 512)
                    S.op("dve", lambda e: e.scalar_tensor_tensor(tt[:, cs], banks[cb], ss[:, 5:6], gt[:, cs], ALU.mult, ALU.mult),
                         r=[bnames[cb], sn, gname], w=[tn])
                S.op("pool", lambda e: e.tensor_tensor(tt[:], tt[:], xs[:], ALU.add), r=[xn, tn], w=[tn])
                S.dma("pool", out_ap, tt[:], tn, r=[tn])

        if on("bias"):
            with ExitStack() as ph:
                oh = sb("oh", [128, 32, 128], F32, ph)
                cm = sb("cm", [128, 128], F32, ph)
                S.dma("sp", cm[:], A["c_cmask"], "cm", w=["cm"])
                for which in range(2):
                    S.dma("sp", oh[:], A["c_oh"][which], "oh", w=["oh"])
                    for h in range(8):
                        eng = "dve"
                        dst = Bm[:, h, which, :]
                        rn = "Bm%d_%d" % (h, which)
                        if which == 0:
                            S.op(eng, lambda e: e.tensor_copy(dst, cm[:]), r=["cm"], w=[rn])
                        else:
                            S.op(eng, lambda e: e.memset(dst, 0.0), w=[rn])
                        for b in range(32):
                            S.op(eng, lambda e: e.scalar_tensor_tensor(dst, oh[:, b, :], tb[:, b * 8 + h:b * 8 + h + 1], dst,
                                                                        ALU.mult, ALU.add), r=["oh", "tb", rn], w=[rn])
                S.barrier()

        for l in range(DEPTH):
            lam_init = 0.8 - 0.6 * math.exp(-0.3 * l)
            for q in range(NSEQ):
                xsrc = A["x"][q] if l == 0 else out[q]

                if on("A"):
                    with ExitStack() as ph:
                        hT = sb("hT", [128, DC, S_], BF16, ph)
                        hres = ["hT%d" % i for i in range(NT)]
                        with ExitStack() as ph1:
                            gpre = load_gamma(ph1, "gpre", A["g_mix_pre"][l])
                            nt = NormT(ph1, "nA")
                            for i in range(NT):
                                nt.run(xsrc[i * 128:(i + 1) * 128, :], "gpre", gpre, hT, hres[i], i * 128)
                            S.barrier()
                        cosT = sb("cosT", [128, S_], F32, ph)
                        sinT = sb("sinT", [128, S_], F32, ph)
                        with ExitStack() as ph2:
                            posi = sb("posi", [128, S_], I32, ph2)
                            ang = sb("ang", [128, S_], F32, ph2)
                            S.dma("sp", posi[:], A["positions"][q].partition_broadcast(128), "posi", w=["posi"])
                            S.op("dve", lambda e: e.tensor_copy(ang[:], posi[:]), r=["posi"], w=["ang"])
                            S.op("dve", lambda e: e.tensor_scalar(ang[:], ang[:], inv_ap, None, ALU.mult), r=["ang", "cvec"], w=["ang"])
                            sincos(sincos_tmp(ph2, "rot", [128, S_]), ang[:], sinT[:], cosT[:], ["ang", "sinT", "cosT"])
                            S.op("dve", lambda e: e.tensor_scalar(sinT[:], sinT[:], sgn_ap, None, ALU.mult), r=["sinT", "cvec"], w=["sinT"])
                            S.barrier()
                        wring = Ring(nc, ph, "wA", [128, DC, 512], BF16, 2)
                        wsw = Ring(nc, ph, "wAs", [128, DC, 512], BF16, 1)
                        pacc = Ring(nc, ph, "pA", [128, 512], F32, 4, psum=True)
                        pswp = Ring(nc, ph, "pAs", [128, 512], F32, 2, psum=True)
                        stg = Ring(nc, ph, "stgA", [128, S_], BF16, 3)
                        stgT = Ring(nc, ph, "stgT", [128, 512], BF16, 3)
                        tmp1 = Ring(nc, ph, "tmpA", [128, 512], F32, 2)
                        tmp2 = Ring(nc, ph, "tmpB", [128, 512], F32, 2)
                        blocks = [("rot", 0), ("rot", 4), ("tm", (rv, 0)), ("silu", 8), ("copy", 12), ("copy", 16), ("copy", 20),
                                  ("copy", 24), ("copy", 28), ("tm", (dv, 0)), ("tm", (dv, 512))]
                        for b, (kind, arg) in enumerate(blocks):
                            wt, wn = wring.next()
                            S.dma("sp", wt[:], Wb["in", l][b], wn, r=["w_in%d" % l], w=[wn])
                            if kind == "rot":
                                ws, wsn = wsw.next()
                                S.dma("sp", ws[:], Wb["insw", l][b], wsn, r=["w_insw%d" % l], w=[wsn])
                            if kind == "tm":
                                dst, c0 = arg
                                for i in range(NT):
                                    pa, pn = pacc.next()
                                    for c in range(DC):
                                        S.op("pe", lambda e: e.matmul(pa[:], hT[:, c, i * 128:(i + 1) * 128], wt[:, c, :],
                                                                       start=(c == 0), stop=(c == DC - 1)), r=[hres[i], wn], w=[pn])
                                    sg, sgn = stgT.next()
                                    eng = "act" if i % 2 else "dve"
                                    if eng == "act":
                                        S.op("act", lambda e: e.copy(sg[:], pa[:]), r=[pn], w=[sgn])
                                    else:
                                        S.op("dve", lambda e: e.tensor_copy(sg[:], pa[:]), r=[pn], w=[sgn])
                                    S.dma("pool", dst[i * 128:(i + 1) * 128, c0:c0 + 512], sg[:], sgn, r=[sgn])
                                continue
                            for j in range(4):
                                sg, sgn = stg.next()
                                for tg in range(NG):
                                    ts = slice(tg * 512, (tg + 1) * 512)
                                    hr = hres[tg * 4:(tg + 1) * 4]
                                    pa, pn = pacc.next()
                                    for c in range(DC):
                                        S.op("pe", lambda e: e.matmul(pa[:], wt[:, c, j * 128:(j + 1) * 128], hT[:, c, ts],
                                                                       start=(c == 0), stop=(c == DC - 1)), r=hr + [wn], w=[pn])
                                    if kind == "rot":
                                        pb, pbn = pswp.next()
                                        for c in range(DC):
                                            S.op("pe", lambda e: e.matmul(pb[:], ws[:, c, j * 128:(j + 1) * 128], hT[:, c, ts],
                                                                           start=(c == 0), stop=(c == DC - 1)), r=hr + [wsn], w=[pbn])
                                        t1, t1n = tmp1.next()
                                        t2, t2n = tmp2.next()
                                        S.op("dve", lambda e: e.tensor_tensor(t1[:], pa[:], cosT[:, ts], ALU.mult), r=[pn, "cosT"], w=[t1n])
                                        S.op("dve", lambda e: e.tensor_tensor(t2[:], pb[:], sinT[:, ts], ALU.mult), r=[pbn, "sinT"], w=[t2n])
                                        S.op("pool", lambda e: e.tensor_tensor(sg[:, ts], t1[:], t2[:], ALU.add), r=[t1n, t2n], w=[sgn])
                                    elif kind == "silu":
                                        S.op("act", lambda e: e.activation(sg[:, ts], pa[:], AF.Silu), r=[pn], w=[sgn])
                                    else:
                                        if (j + tg) % 2:
                                            S.op("act", lambda e: e.copy(sg[:, ts], pa[:]), r=[pn], w=[sgn])
                                        else:
                                            S.op("dve", lambda e: e.tensor_copy(sg[:, ts], pa[:]), r=[pn], w=[sgn])
                                S.dma("pool", fmT[arg + j], sg[:], sgn, r=[sgn])
                        S.barrier()

                if on("B"):
                    with ExitStack() as ph:
                        decay = sb("decay", [128, 4, 128], F32, ph)
                        xi = sb("xi", [128, 4, 128], F32, ph)
                        zeta = sb("zeta", [128, 4], F32, ph)
                        gch = sb("gch", [128, 4], F32, ph)
                        for nm_, t_ in (("decay", decay), ("xi", xi), ("zeta", zeta), ("gch", gch)):
                            S.dma("sp", t_[:], A["c_" + nm_], "rc_" + nm_, w=["rc_" + nm_])
                        qT = sb("rqT", [128, 4, S_], BF16, ph)
                        kT = sb("rkT", [128, 4, S_], BF16, ph)
                        gT = sb("rgT", [128, 4, S_], BF16, ph)
                        qx = sb("rqx", [128, 4, S_], BF16, ph)
                        vv = sb("rvv", [128, NT, 512], BF16, ph)
                        yst = sb("ryst", [128, 4, S_], BF16, ph)
                        R32 = sb("R32", [128, 4, 128], F32, ph)
                        Rb = sb("Rb", [128, 4, 128], BF16, ph)
                        for h in range(4):
                            S.dma("sp", qT[:, h, :], fmT[h], "rq%d" % h, w=["rq%d" % h])
                            S.dma("sp", kT[:, h, :], fmT[4 + h], "rk%d" % h, w=["rk%d" % h])
                            S.dma("sp", gT[:, h, :], fmT[8 + h], "rg%d" % h, w=["rg%d" % h])
                        S.dma("sp", vv[:], rv.rearrange("(i p) e -> p i e", p=128), "rvv", w=["rvv"])
                        for h in range(4):
                            S.op("pool", lambda e: e.tensor_tensor(qx[:, h, :].rearrange("p (c t) -> p c t", t=128),
                                                                   qT[:, h, :].rearrange("p (c t) -> p c t", t=128),
                                                                   xi[:, h, :].unsqueeze(1).broadcast_to([128, NT, 128]), ALU.mult),
                                 r=["rq%d" % h, "rc_xi"], w=["rqx%d" % h])
                        pBa = ps("pBa", [128, 4, 128], F32, ph)
                        pBb = ps("pBb", [128, 4, 128], BF16, ph)
                        p_in = VRing([pBa[:, 0, :], pBa[:, 1, :]], ["pBi0", "pBi1"])
                        pBc = ps("pBc", [128, 4, 128], F32, ph)
                        p_dr = VRing([pBc[:, 0, :], pBc[:, 2, :]], ["pBd0", "pBd1"])
                        R32b = sb("R32b", [128, 4, 128], F32, ph)
                        p_kt = VRing([pBb[:, 0, :], pBb[:, 1, :]], ["pBk0", "pBk1"])
                        p_o = [ps("pBo%d" % h, [128, 512], F32, ph) for h in range(4)]
                        p_ss = ps("pBss", [128, 512], F32, ph)
                        msk = Ring(nc, ph, "mskB", [128, 128], BF16, 3)
                        kz = Ring(nc, ph, "kzB", [128, 128], BF16, 2)
                        sq = Ring(nc, ph, "sqB", [128, 512], BF16, 2)
                        rt = Ring(nc, ph, "rtB", [128, 512], F32, 2)
                        tB = Ring(nc, ph, "tB", [128, 512], F32, 2)
                        for c in range(NT if cfg.bcut >= 2 else 0):
                            cs = slice(c * 128, (c + 1) * 128)
                            oc = slice((c % 4) * 128, (c % 4) * 128 + 128)
                            for h in range(4):
                                pi, pin = p_in.next()
                                S.op("pe", lambda e: e.matmul(pi[:], kT[:, h, cs], qT[:, h, cs], start=True, stop=True),
                                     r=["rk%d" % h, "rq%d" % h], w=[pin])
                                mk, mkn = msk.next()
                                S.op("dve", lambda e: e.tensor_tensor(mk[:], pi[:], decay[:, h, :], ALU.mult), r=[pin, "rc_decay"], w=[mkn])
                                S.op("pe", lambda e: e.matmul(p_o[h][:, oc], vv[:, c, h * 128:(h + 1) * 128], mk[:], start=True, stop=(c == 0)),
                                     r=["rvv", mkn], w=["pBo%d" % h])
                                if c > 0 and cfg.bcut >= 3:
                                    S.op("pe", lambda e: e.matmul(p_o[h][:, oc], Rb[:, h, :], qx[:, h, cs], start=False, stop=True),
                                         r=["Rb%d" % h, "rqx%d" % h], w=["pBo%d" % h])
                                if c < NT - 1 and cfg.bcut >= 3:
                                    pk, pkn = p_kt.next()
                                    S.op("pe", lambda e: e.transpose(pk[:], kT[:, h, cs], ident[:]), r=["rk%d" % h, "ident"], w=[pkn])
                                    kzt, kzn = kz.next()
                                    S.op("dve", lambda e: e.tensor_scalar(kzt[:], pk[:], zeta[:, h:h + 1], None, ALU.mult), r=[pkn, "rc_zeta"], w=[kzn])
                                    pd, pdn = p_dr.next()
                                    S.op("pe", lambda e: e.matmul(pd[:], kzt[:], vv[:, c, h * 128:(h + 1) * 128], start=True, stop=True),
                                         r=[kzn, "rvv"], w=[pdn])
                                    Rn = (R32, R32b)[c % 2]
                                    Ro = (R32, R32b)[(c + 1) % 2]
                                    if c == 0:
                                        S.op("dve", lambda e: e.tensor_copy(Rn[:, h, :], pd[:]), r=[pdn], w=["R32%d" % h])
                                    else:
                                        S.op("dve", lambda e: e.tensor_scalar(Rn[:, h, :], Ro[:, h, :], gch[:, h:h + 1], None, ALU.mult),
                                             r=["R32%d" % h, "rc_gch"], w=["R32%d" % h])
                                        S.op("dve", lambda e: e.tensor_tensor(Rn[:, h, :], Rn[:, h, :], pd[:], ALU.add), r=[pdn, "R32%d" % h], w=["R32%d" % h])
                                    S.op("dve", lambda e: e.tensor_copy(Rb[:, h, :], Rn[:, h, :]), r=["R32%d" % h], w=["Rb%d" % h])
                                if c % 4 == 3 and cfg.bcut >= 4:
                                    ts = slice((c - 3) * 128, (c + 1) * 128)
                                    sqt, sqn = sq.next()
                                    rtt, rtn = rt.next()
                                    tt, tn = tB.next()
                                    S.op("act", lambda e: e.activation(sqt[:], p_o[h][:], AF.Square), r=["pBo%d" % h], w=[sqn])
                                    S.op("pe", lambda e: e.matmul(p_ss[:], ones[:], sqt[:], start=True, stop=True), r=[sqn, "ones"], w=["pBss"])
                                    S.op("act", lambda e: e.activation(rtt[:], p_ss[:], AF.Sqrt, bias=eps_ap, scale=1.0 / 128), r=["pBss", "cvec"], w=[rtn])
                                    S.op("dve", lambda e: e.reciprocal(rtt[:], rtt[:]), r=[rtn], w=[rtn])
                                    S.op("dve", lambda e: e.tensor_tensor(tt[:], p_o[h][:], rtt[:], ALU.mult), r=["pBo%d" % h, rtn], w=[tn])
                                    S.op("pool", lambda e: e.tensor_tensor(yst[:, h, ts], tt[:], gT[:, h, ts], ALU.mult),
                                         r=[tn, "rg%d" % h], w=["ryst%d" % h])
                        for h in range(4):
                            S.dma("pool", yT[h], yst[:, h, :], "ryst%d" % h, r=["ryst%d" % h])
                        S.barrier()

                if on("C"):
                    with ExitStack() as ph:
                        LB = [sb("LB%d" % i, [128, 16, 128], BF16, ph) for i in range(2)]
                        LC = [sb("LC%d" % i, [128, 16, 128], BF16, ph) for i in range(2)]
                        th = sb("s5th", [128, 16], F32, ph)
                        mag = sb("s5mag", [128, 16], F32, ph)
                        cL = sb("s5cL", [128, 16], F32, ph)
                        sL = sb("s5sL", [128, 16], F32, ph)
                        dsk = sb("s5dsk", [128, 4], F32, ph)
                        wgl = sb("s5wgl", [128, 4, 512], BF16, ph)
                        S.dma("sp", wgl[:], Wb["glu", l], "s5wgl", r=["w_x%d" % l], w=["s5wgl"])
                        S.dma("sp", dsk[:], A["d_skip"][l].rearrange("(c p) -> p c", p=128), "s5dsk", w=["s5dsk"], allow_slow_non_contiguous=True)
                        with ExitStack() as p1:
                            lr = sb("s5lr", [128, 16], F32, p1)
                            li = sb("s5li", [128, 16], F32, p1)
                            dt = sb("s5dt", [128, 16], F32, p1)
                            S.dma("sp", lr[:], A["lam_re"][l].rearrange("(k g2) p -> (g2 p) k", g2=2), "s5lr", w=["s5s"], allow_slow_non_contiguous=True)
                            S.dma("sp", li[:], A["lam_im"][l].rearrange("(k g2) p -> (g2 p) k", g2=2), "s5li", w=["s5s"], allow_slow_non_contiguous=True)
                            ldv = A["log_dt"][l].rearrange("(k g2) -> g2 k", g2=2)
                            for g2 in range(2):
                                S.dma("sp", dt[g2 * 64:(g2 + 1) * 64, :], ldv[g2:g2 + 1, :].broadcast_to([64, 16]), "s5dt", w=["s5s"],
                                      allow_slow_non_contiguous=True)
                            sm = ["s5s"]
                            t16 = [sb("s5t%d" % i, [128, 16], F32, p1) for i in range(8)]
                            abr, abi, nr, den, fre, fim, u0, u1 = t16
                            V = lambda fn: S.op("dve", fn, r=sm + ["cvec"], w=sm)
                            S.op("act", lambda e: e.activation(dt[:], dt[:], AF.Exp), r=sm, w=sm)
                            V(lambda e: e.tensor_scalar(lr[:], lr[:], -1e-4, None, ALU.min))
                            V(lambda e: e.tensor_tensor(u0[:], lr[:], dt[:], ALU.mult))
                            S.op("act", lambda e: e.activation(mag[:], u0[:], AF.Exp), r=sm, w=sm)
                            V(lambda e: e.tensor_tensor(th[:], li[:], dt[:], ALU.mult))
                            sc16 = sincos_tmp(p1, "s5a", [128, 16])
                            sincos(sc16, th[:], abi[:], abr[:], sm)
                            V(lambda e: e.tensor_tensor(abr[:], abr[:], mag[:], ALU.mult))
                            V(lambda e: e.tensor_tensor(abi[:], abi[:], mag[:], ALU.mult))
                            V(lambda e: e.tensor_scalar(nr[:], abr[:], -1.0, None, ALU.add))
                            V(lambda e: e.tensor_tensor(den[:], lr[:], lr[:], ALU.mult))
                            V(lambda e: e.tensor_tensor(u0[:], li[:], li[:], ALU.mult))
                            V(lambda e: e.tensor_tensor(den[:], den[:], u0[:], ALU.add))
                            V(lambda e: e.reciprocal(den[:], den[:]))
                            V(lambda e: e.tensor_tensor(u0[:], nr[:], lr[:], ALU.mult))
                            V(lambda e: e.tensor_tensor(u1[:], abi[:], li[:], ALU.mult))
                            V(lambda e: e.tensor_tensor(fre[:], u0[:], u1[:], ALU.add))
                            V(lambda e: e.tensor_tensor(fre[:], fre[:], den[:], ALU.mult))
                            V(lambda e: e.tensor_tensor(u0[:], abi[:], lr[:], ALU.mult))
                            V(lambda e: e.tensor_tensor(u1[:], nr[:], li[:], ALU.mult))
                            V(lambda e: e.tensor_tensor(fim[:], u0[:], u1[:], ALU.subtract))
                            V(lambda e: e.tensor_tensor(fim[:], fim[:], den[:], ALU.mult))
                            V(lambda e: e.tensor_scalar(u0[:], th[:], 512.0, None, ALU.mult))
                            sincos(sc16, u0[:], sL[:], cL[:], sm)
                            bre = sb("s5bre", [128, 16, 16], F32, p1)
                            bim = sb("s5bim", [128, 16, 16], F32, p1)
                            S.dma("sp", bre[:], A["b_re"][l].rearrange("(k g2) p h -> (g2 p) k h", g2=2), "s5bre", w=sm)
                            S.dma("sp", bim[:], A["b_im"][l].rearrange("(k g2) p h -> (g2 p) k h", g2=2), "s5bim", w=sm)
                            bbr = sb("s5bbr", [128, 16, 16], F32, p1)
                            bbi = sb("s5bbi", [128, 16, 16], F32, p1)
                            v0 = sb("s5v0", [128, 16, 16], F32, p1)
                            fr3 = fre[:].unsqueeze(2).broadcast_to([128, 16, 16])
                            fi3 = fim[:].unsqueeze(2).broadcast_to([128, 16, 16])
                            V(lambda e: e.tensor_tensor(bbr[:], bre[:], fr3, ALU.mult))
                            V(lambda e: e.tensor_tensor(v0[:], bim[:], fi3, ALU.mult))
                            V(lambda e: e.tensor_tensor(bbr[:], bbr[:], v0[:], ALU.subtract))
                            V(lambda e: e.tensor_tensor(bbi[:], bim[:], fr3, ALU.mult))
                            V(lambda e: e.tensor_tensor(v0[:], bre[:], fi3, ALU.mult))
                            V(lambda e: e.tensor_tensor(bbi[:], bbi[:], v0[:], ALU.add))
                            xpad = sb("s5xpad", [128, 16, 128], BF16, p1)
                            ptp = Ring(nc, p1, "s5ptp", [128, 128], BF16, 2, psum=True)
                            for ri, bb in enumerate((bbr, bbi)):
                                V(lambda e: e.memset(xpad[:], 0.0))
                                for k in range(16):
                                    off = 32 * (k % 4)
                                    V(lambda e: e.tensor_scalar(xpad[:, k, off:off + 16], bb[:, k, :], m0_ap, None, ALU.mult))
                                    V(lambda e: e.tensor_scalar(xpad[:, k, off + 16:off + 32], bb[:, k, :], m1_ap, None, ALU.mult))
                                for k in range(16):
                                    pt_, ptn = ptp.next()
                                    S.op("pe", lambda e: e.transpose(pt_[:], xpad[:, k, :], ident[:]), r=sm + ["ident"], w=[ptn])
                                    S.op("act", lambda e: e.copy(LB[ri][:, k, :], pt_[:]), r=[ptn], w=["LB"])
                            s5mask = sb("s5mask", [128, 4, 128], F32, p1)
                            S.dma("sp", s5mask[:], A["c_s5mask"], "s5mask", w=sm)
                            c32 = sb("s5c32", [128, 4, 64], F32, p1)
                            cdup = sb("s5cdup", [128, 4, 128], BF16, p1)
                            for ri, nm_ in enumerate(("c_re", "c_im")):
                                S.dma("sp", c32[:], A[nm_][l].rearrange("(uc gl) h p -> (gl h) uc p", gl=8), "s5c32", w=sm)
                                V(lambda e: e.tensor_copy(cdup[:, :, 0:64], c32[:]))
                                V(lambda e: e.tensor_copy(cdup[:, :, 64:128], c32[:]))
                                for uc in range(4):
                                    pt_, ptn = ptp.next()
                                    S.op("pe", lambda e: e.transpose(pt_[:], cdup[:, uc, :], ident[:]), r=sm + ["ident"], w=[ptn])
                                    for kk in range(4):
                                        k = uc * 4 + kk
                                        S.op("dve", lambda e: e.scalar_tensor_tensor(LC[ri][:, k, :], pt_[:], (1.0 if ri == 0 else -1.0),
                                                                                     s5mask[:, kk, :], ALU.mult, ALU.mult), r=[ptn] + sm, w=["LC"])
                            S.barrier()
                        uT = sb("s5uT", [128, 4, S_], BF16, ph)
                        gTt = sb("s5gT", [128, 4, S_], BF16, ph)
                        for uc in range(4):
                            S.dma("sp", uT[:, uc, :], fmT[12 + uc], "s5u%d" % uc, w=["s5u%d" % uc])
                        iota = sb("s5iota", [128, 512], F32, ph)
                        S.dma("sp", iota[:], A["c_iota512"], "s5iota", w=["s5iota"])
                        tabC = sb("s5tabC", [128, 4, 512], F32, ph)
                        tabS = sb("s5tabS", [128, 4, 512], F32, ph)
                        rtab = sb("s5rtab", [128, 4, 512], F32, ph)
                        angk = sb("s5angk", [128, 512], F32, ph)
                        ini = sb("s5ini", [128, 4, 2], F32, ph)
                        p_bu = Ring(nc, ph, "pCb", [128, 2, 512], F32, 2, psum=True)
                        p_y = Ring(nc, ph, "pCy", [128, 512], F32, 2, psum=True)
                        p_gl = Ring(nc, ph, "pCg", [128, 512], F32, 2, psum=True)
                        m_t = [Ring(nc, ph, "s5m%d" % i, [128, 512], F32, 2) for i in range(4)]
                        mre = Ring(nc, ph, "s5mre", [128, 512], F32, 2)
                        mim = Ring(nc, ph, "s5mim", [128, 512], F32, 2)
                        zre = Ring(nc, ph, "s5zre", [128, 512], F32, 2)
                        zim = Ring(nc, ph, "s5zim", [128, 512], F32, 2)
                        d_t = [Ring(nc, ph, "s5d%d" % i, [128, 512], F32, 2) for i in range(4)]
                        xre = Ring(nc, ph, "s5xre", [128, 512], BF16, 2)
                        xim = Ring(nc, ph, "s5xim", [128, 512], BF16, 2)
                        yv = Ring(nc, ph, "s5yv", [128, 512], F32, 2)
                        e1 = Ring(nc, ph, "s5e1", [128, 512], F32, 2)
                        e2 = Ring(nc, ph, "s5e2", [128, 512], F32, 2)
                        cr = sb("s5cr", [128, 4], F32, ph)
                        sc512 = sincos_tmp(ph, "s5c", [128, 512])
                        TT = ALU
                        for uc in range(4):
                            for kk in range(4):
                                k = uc * 4 + kk
                                tn_ = "s5tab%d" % kk
                                S.op("dve", lambda e: e.tensor_scalar(angk[:], iota[:], th[:, k:k + 1], None, ALU.mult), r=["s5iota", "s5s"], w=["s5angk"])
                                sincos(sc512, angk[:], tabS[:, kk, :], tabC[:, kk, :], ["s5angk", tn_])
                                S.op("dve", lambda e: e.memset(rtab[:, kk, :], 1.0), w=[tn_ + "r"])
                                S.op("dve", lambda e: e.tensor_scalar(rtab[:, kk, :], rtab[:, kk, :], mag[:, k:k + 1], None, ALU.mult), r=["s5s"], w=[tn_ + "r"])
                            for j in range(NG):
                                ts = slice(j * 512, (j + 1) * 512)
                                py, pyn = p_y.next()
                                for kk in range(4):
                                    k = uc * 4 + kk
                                    tn_ = "s5tab%d" % kk
                                    Ck, Sk = tabC[:, kk, :], tabS[:, kk, :]
                                    pb, pbn = p_bu.next()
                                    for ri in range(2):
                                        S.op("pe", lambda e: e.matmul(pb[:, ri, :], LB[ri][:, k, :], uT[:, uc, ts], start=True, stop=True),
                                             r=["LB", "s5u%d" % uc], w=[pbn])
                                    ms = [r_.next() for r_ in m_t]
                                    (a0, a0n), (a1, a1n), (a2, a2n), (a3, a3n) = ms
                                    mr, mrn = mre.next()
                                    mi, min_ = mim.next()
                                    S.op("dve", lambda e: e.tensor_tensor(a0[:], pb[:, 0, :], Ck, TT.mult), r=[pbn, tn_], w=[a0n])
                                    S.op("dve", lambda e: e.tensor_tensor(a1[:], pb[:, 1, :], Sk, TT.mult), r=[pbn, tn_], w=[a1n])
                                    S.op("pool", lambda e: e.tensor_tensor(mr[:], a0[:], a1[:], TT.add), r=[a0n, a1n], w=[mrn])
                                    S.op("dve", lambda e: e.tensor_tensor(a2[:], pb[:, 1, :], Ck, TT.mult), r=[pbn, tn_], w=[a2n])
                                    S.op("dve", lambda e: e.tensor_tensor(a3[:], pb[:, 0, :], Sk, TT.mult), r=[pbn, tn_], w=[a3n])
                                    S.op("pool", lambda e: e.tensor_tensor(mi[:], a2[:], a3[:], TT.subtract), r=[a2n, a3n], w=[min_])
                                    zr, zrn = zre.next()
                                    zi, zin = zim.next()
                                    inr = 0.0 if j == 0 else ini[:, kk, 0:1]
                                    ini_ = 0.0 if j == 0 else ini[:, kk, 1:2]
                                    inn = "s5ini%d" % kk
                                    S.op("dve", lambda e: e.tensor_tensor_scan(zr[:], rtab[:, kk, :], mr[:], inr, TT.mult, TT.add),
                                         r=[tn_ + "r", mrn, inn], w=[zrn])
                                    S.op("dve", lambda e: e.tensor_tensor_scan(zi[:], rtab[:, kk, :], mi[:], ini_, TT.mult, TT.add),
                                         r=[tn_ + "r", min_, inn], w=[zin])
                                    if j < NG - 1:
                                        S.op("dve", lambda e: e.tensor_tensor(cr[:, 0:1], zi[:, 511:512], sL[:, k:k + 1], TT.mult), r=[zin, "s5s"], w=["s5cr"])
                                        S.op("dve", lambda e: e.scalar_tensor_tensor(ini[:, kk, 0:1], zr[:, 511:512], cL[:, k:k + 1], cr[:, 0:1], TT.mult, TT.subtract),
                                             r=[zrn, "s5cr", "s5s"], w=[inn])
                                        S.op("dve", lambda e: e.tensor_tensor(cr[:, 1:2], zr[:, 511:512], sL[:, k:k + 1], TT.mult), r=[zrn, "s5s"], w=["s5cr"])
                                        S.op("dve", lambda e: e.scalar_tensor_tensor(ini[:, kk, 1:2], zi[:, 511:512], cL[:, k:k + 1], cr[:, 1:2], TT.mult, TT.add),
                                             r=[zin, "s5cr", "s5s"], w=[inn])
                                    ds = [r_.next() for r_ in d_t]
                                    (b0, b0n), (b1, b1n), (b2, b2n), (b3, b3n) = ds
                                    xr, xrn = xre.next()
                                    xi_, xin_ = xim.next()
                                    S.op("pool", lambda e: e.tensor_tensor(b0[:], zr[:], Ck, TT.mult), r=[zrn, tn_], w=[b0n])
                                    S.op("pool", lambda e: e.tensor_tensor(b1[:], zi[:], Sk, TT.mult), r=[zin, tn_], w=[b1n])
                                    S.op("pool", lambda e: e.tensor_tensor(xr[:], b0[:], b1[:], TT.subtract), r=[b0n, b1n], w=[xrn])
                                    S.op("pool", lambda e: e.tensor_tensor(b2[:], zi[:], Ck, TT.mult), r=[zin, tn_], w=[b2n])
                                    S.op("pool", lambda e: e.tensor_tensor(b3[:], zr[:], Sk, TT.mult), r=[zrn, tn_], w=[b3n])
                                    S.op("pool", lambda e: e.tensor_tensor(xi_[:], b2[:], b3[:], TT.add), r=[b2n, b3n], w=[xin_])
                                    S.op("pe", lambda e: e.matmul(py[:], LC[0][:, k, :], xr[:], start=(kk == 0), stop=False), r=["LC", xrn], w=[pyn])
                                    S.op("pe", lambda e: e.matmul(py[:], LC[1][:, k, :], xi_[:], start=False, stop=(kk == 3)), r=["LC", xin_], w=[pyn])
                                yt_, ytn = yv.next()
                                f1, f1n = e1.next()
                                f2, f2n = e2.next()
                                S.op("dve", lambda e: e.scalar_tensor_tensor(yt_[:], uT[:, uc, ts], dsk[:, uc:uc + 1], py[:], TT.mult, TT.add),
                                     r=["s5u%d" % uc, "s5dsk", pyn], w=[ytn])
                                S.op("act", lambda e: e.activation(f1[:], yt_[:], AF.Square), r=[ytn], w=[f1n])
                                S.op("dve", lambda e: e.tensor_scalar(f1[:], f1[:], 0.044715, 1.0, TT.mult, TT.add), r=[f1n], w=[f1n])
                                S.op("dve", lambda e: e.tensor_tensor(f1[:], f1[:], yt_[:], TT.mult), r=[f1n, ytn], w=[f1n])
                                S.op("act", lambda e: e.activation(f2[:], f1[:], AF.Sigmoid, scale=2.0 * math.sqrt(2.0 / math.pi)), r=[f1n], w=[f2n])
                                S.op("pool", lambda e: e.tensor_tensor(gTt[:, uc, ts], yt_[:], f2[:], TT.mult), r=[ytn, f2n], w=["s5g%d_%d" % (uc, j)])
                        yst = sb("s5yst", [128, 4, S_], BF16, ph)
                        sg_ = Ring(nc, ph, "s5sg", [128, 512], F32, 2)
                        for j in range(NG):
                            ts = slice(j * 512, (j + 1) * 512)
                            for oc in range(4):
                                pg, pgn = p_gl.next()
                                for ci in range(4):
                                    S.op("pe", lambda e: e.matmul(pg[:], wgl[:, ci, oc * 128:(oc + 1) * 128], gTt[:, ci, ts], start=(ci == 0), stop=(ci == 3)),
                                         r=["s5wgl", "s5g%d_%d" % (ci, j)], w=[pgn])
                                sgt, sgn = sg_.next()
                                S.op("act", lambda e: e.activation(sgt[:], pg[:], AF.Sigmoid), r=[pgn], w=[sgn])
                                S.op("dve", lambda e: e.tensor_tensor(yst[:, oc, ts], gTt[:, oc, ts], sgt[:], TT.mult), r=[sgn, "s5g%d_%d" % (oc, j)], w=["s5yst%d" % oc])
                        for oc in range(4):
                            S.dma("pool", yT[4 + oc], yst[:, oc, :], "s5yst%d" % oc, r=["s5yst%d" % oc])
                        S.barrier()

                if q == 0 and l + 1 < DEPTH:
                    prepass(l + 1)

                if on("D"):
                    with ExitStack() as ph:
                        scale = 64 ** -0.5
                        lq = sb("dlq", [128, 4, 64], F32, ph)
                        for i_, nm_ in enumerate(("lam_q1", "lam_k1", "lam_q2", "lam_k2")):
                            S.dma("sp", lq[:, i_, :], A[nm_][l].partition_broadcast(128), "dlq", w=["dsc"])
                        sc = sb("dsc", [128, 8], F32, ph)
                        pr = sb("dpr", [128, 2, 64], F32, ph)
                        gs = sb("dgs", [128, 1], F32, ph)
                        S.dma("sp", gs[:], A["g_subln"][l].rearrange("(p o) -> p o", o=1), "dgs", w=["dsc"])
                        V = lambda fn: S.op("dve", fn, r=["dsc"], w=["dsc"])
                        V(lambda e: e.tensor_tensor(pr[:, 0, :], lq[:, 0, :], lq[:, 1, :], ALU.mult))
                        V(lambda e: e.tensor_tensor(pr[:, 1, :], lq[:, 2, :], lq[:, 3, :], ALU.mult))
                        V(lambda e: e.tensor_reduce(sc[:, 0:2], pr[:], AX.X, ALU.add))
                        S.op("act", lambda e: e.activation(sc[:, 0:2], sc[:, 0:2], AF.Exp), r=["dsc"], w=["dsc"])
                        V(lambda e: e.tensor_tensor(sc[:, 2:3], sc[:, 1:2], sc[:, 0:1], ALU.subtract))
                        V(lambda e: e.tensor_scalar(sc[:, 2:3], sc[:, 2:3], -lam_init, None, ALU.add))
                        V(lambda e: e.tensor_scalar(gs[:], gs[:], 1.0 - lam_init, None, ALU.mult))
                        neglam = sc[:, 2:3]
                        qh = Ring(nc, ph, "dqh", [128, S_], BF16, 2)
                        kh = Ring(nc, ph, "dkh", [128, S_], BF16, 2)
                        vh = Ring(nc, ph, "dvh", [128, NT, 128], BF16, 2)
                        yst = Ring(nc, ph, "dyst", [128, S_], BF16, 2)
                        p_st = Ring(nc, ph, "pDs", [128, 512], F32, 3, psum=True)
                        p_o = [ps("pDo%d" % m, [128, 512], F32, ph) for m in range(2)]
                        p_d = [ps("pDd%d" % m, [128, 512], F32, ph) for m in range(2)]
                        p_ss = ps("pDss", [128, 512], F32, ph)
                        PT = Ring(nc, ph, "dPT", [128, 512], BF16, 4)
                        tmpb = Ring(nc, ph, "dtb", [128, 128], F32, 3)
                        r1 = Ring(nc, ph, "dr1", [128, 512], F32, 2)
                        o1 = Ring(nc, ph, "do1", [128, 512], F32, 2)
                        o2 = Ring(nc, ph, "do2", [128, 512], F32, 2)
                        sqd = Ring(nc, ph, "dsq", [128, 512], BF16, 2)
                        dvv = dv.rearrange("(i p) e -> p i e", p=128)
                        for h in range(8):
                            q_, qn = qh.next()
                            k_, kn = kh.next()
                            v_, vn = vh.next()
                            ys, ysn = yst.next()
                            S.dma("sp", q_[:], fmT[16 + h], qn, w=[qn])
                            S.dma("sp", k_[:], fmT[24 + h], kn, w=[kn])
                            S.dma("sp", v_[:], dvv[:, :, h * 128:(h + 1) * 128], vn, w=[vn])
                            cb_ap = tb[:, 31 * 8 + h:31 * 8 + h + 1]
                            for cq in range(NG):
                                njb = 4 * (cq + 1)
                                for j in range(njb):
                                    jj = j - 4 * cq
                                    ii0 = max(0, jj)
                                    c0 = ii0 * 128
                                    for m in range(2):
                                        ms = slice(64 * m, 64 * m + 64)
                                        pst, pstn = p_st.next()
                                        S.op("pe", lambda e: e.matmul(pst[:, c0:512], k_[ms, j * 128:(j + 1) * 128], q_[ms, cq * 512 + c0:(cq + 1) * 512],
                                                                       start=True, stop=True), r=[kn, qn], w=[pstn])
                                        pt_, ptn = PT.next()
                                        for ii in range(ii0, 4):
                                            d = 4 * cq + ii - j
                                            if d >= 2:
                                                break
                                            tb_, tbn = tmpb.next()
                                            isl = slice(ii * 128, (ii + 1) * 128)
                                            S.op("dve", lambda e: e.scalar_tensor_tensor(tb_[:], pst[:, isl], scale, Bm[:, h, d, :], ALU.mult, ALU.add),
                                                 r=[pstn, "Bm%d_%d" % (h, d)], w=[tbn])
                                            S.op("act", lambda e: e.activation(pt_[:, isl], tb_[:], AF.Exp), r=[tbn], w=[ptn])
                                        iic = max(ii0, j + 2 - 4 * cq)
                                        if iic < 4:
                                            S.op("act", lambda e: e.activation(pt_[:, iic * 128:512], pst[:, iic * 128:512], AF.Exp, bias=cb_ap, scale=scale),
                                                 r=[pstn, "tb"], w=[ptn])
                                        S.op("pe", lambda e: e.matmul(p_o[m][:, c0:512], v_[:, j, :], pt_[:, c0:512], start=(j == 0), stop=(j == njb - 1)),
                                             r=[vn, ptn], w=["pDo%d" % m])
                                        S.op("pe", lambda e: e.matmul(p_d[m][:, c0:512], ones[:], pt_[:, c0:512], start=(j == 0), stop=(j == njb - 1)),
                                             r=["ones", ptn], w=["pDd%d" % m])
                                ts = slice(cq * 512, (cq + 1) * 512)
                                ra, ran = r1.next()
                                oa, oan = o1.next()
                                ob, obn = o2.next()
                                S.op("dve", lambda e: e.reciprocal(ra[:], p_d[0][:]), r=["pDd0"], w=[ran])
                                S.op("dve", lambda e: e.tensor_tensor(oa[:], p_o[0][:], ra[:], ALU.mult), r=["pDo0", ran], w=[oan])
                                S.op("dve", lambda e: e.reciprocal(ra[:], p_d[1][:]), r=["pDd1", ran], w=[ran])
                                S.op("dve", lambda e: e.tensor_tensor(ob[:], p_o[1][:], ra[:], ALU.mult), r=["pDo1", ran], w=[obn])
                                S.op("dve", lambda e: e.scalar_tensor_tensor(oa[:], ob[:], neglam, oa[:], ALU.mult, ALU.add), r=[obn, oan, "dsc"], w=[oan])
                                sq_, sqn = sqd.next()
                                S.op("act", lambda e: e.activation(sq_[:], oa[:], AF.Square), r=[oan], w=[sqn])
                                S.op("pe", lambda e: e.matmul(p_ss[:], ones[:], sq_[:], start=True, stop=True), r=["ones", sqn], w=["pDss"])
                                S.op("act", lambda e: e.activation(ra[:], p_ss[:], AF.Sqrt, bias=eps_ap, scale=1.0 / 128), r=["pDss", "cvec", ran], w=[ran])
                                S.op("dve", lambda e: e.reciprocal(ra[:], ra[:]), r=[ran], w=[ran])
                                S.op("dve", lambda e: e.scalar_tensor_tensor(ys[:, ts], oa[:], gs[:, 0:1], ra[:], ALU.mult, ALU.mult), r=[oan, ran, "dsc"], w=[ysn])
                            S.dma("pool", yT[8 + h], ys[:], ysn, r=[ysn])
                        S.barrier()

                if on("E"):
                    with ExitStack() as ph:
                        wo = sb("woE", [128, DC, D], BF16, ph)
                        for b in range(4):
                            S.dma("sp", wo[:, :, b * 512:(b + 1) * 512], Wb["out", l][b], "woE", r=["w_out%d" % l], w=["woE"])
                        gpost = load_gamma(ph, "gpostE", A["g_mix_post"][l])
                        yg = Ring(nc, ph, "ygE", [128, DC, 512], BF16, 2)
                        banks = Ring(nc, ph, "pE", [128, 512], F32, 8, psum=True)
                        tail = Tail(ph, "tE")
                        for tg in range(NG):
                            y_, yn = yg.next()
                            S.dma("sp", y_[:], yT[:, :, tg * 512:(tg + 1) * 512].rearrange("c p t -> p c t"), yn, w=[yn])
                            for ii in range(4):
                                i = tg * 4 + ii
                                bk = [banks.next() for _ in range(4)]
                                for cb in range(4):
                                    for c in range(DC):
                                        S.op("pe", lambda e: e.matmul(bk[cb][0][:], y_[:, c, ii * 128:(ii + 1) * 128], wo[:, c, cb * 512:(cb + 1) * 512],
                                                                       start=(c == 0), stop=(c == DC - 1)), r=[yn, "woE"], w=[bk[cb][1]])
                                tail.run([b_[0][:] for b_ in bk], [b_[1] for b_ in bk], "gpostE", gpost,
                                         xsrc[i * 128:(i + 1) * 128, :], out[q][i * 128:(i + 1) * 128, :])
                        S.barrier()

                if on("F"):
                    with ExitStack() as ph:
                        wq = sb("wqF", [128, DC, 512], BF16, ph)
                        wo = sb("woF", [128, 4, D], BF16, ph)
                        S.dma("sp", wq[:], Wb["xq", l], "wqF", r=["w_x%d" % l], w=["wqF"])
                        S.dma("sp", wo[:], Wb["xo", l], "woF", r=["w_x%d" % l], w=["woF"])
                        kxT = sb("kxT", [128, 4, MEM], BF16, ph)
                        vx = sb("vx", [128, MT, 512], BF16, ph)
                        gpre = load_gamma(ph, "gpreF", A["g_x_pre"][l])
                        gpost = load_gamma(ph, "gpostF", A["g_x_post"][l])
                        with ExitStack() as p1:
                            gm = load_gamma(p1, "gmem", A["g_mem"][l])
                            wkv = sb("wkvF", [128, DC, 1024], BF16, p1)
                            for b in range(2):
                                S.dma("sp", wkv[:, :, b * 512:(b + 1) * 512], Wb["xkv", l][b], "wkvF", r=["w_x%d" % l], w=["wkvF"])
                            memT = sb("memT", [128, DC, MEM], BF16, p1)
                            ntm = NormT(p1, "nFm")
                            for mt in range(MT):
                                ntm.run(A["mem"][q][mt * 128:(mt + 1) * 128, :], "gmem", gm, memT, "memT", mt * 128)
                            pk = Ring(nc, p1, "pFk", [128, 512], F32, 2, psum=True)
                            for h in range(4):
                                p_, pn = pk.next()
                                for c in range(DC):
                                    S.op("pe", lambda e: e.matmul(p_[:, 0:MEM], wkv[:, c, h * 128:(h + 1) * 128], memT[:, c, :], start=(c == 0), stop=(c == DC - 1)),
                                         r=["wkvF", "memT"], w=[pn])
                                S.op("act", lambda e: e.copy(kxT[:, h, :], p_[:, 0:MEM]), r=[pn], w=["kxT"])
                            for mt in range(MT):
                                p_, pn = pk.next()
                                for c in range(DC):
                                    S.op("pe", lambda e: e.matmul(p_[:], memT[:, c, mt * 128:(mt + 1) * 128], wkv[:, c, 512:1024], start=(c == 0), stop=(c == DC - 1)),
                                         r=["wkvF", "memT"], w=[pn])
                                S.op("dve", lambda e: e.tensor_copy(vx[:, mt, :], p_[:]), r=[pn], w=["vx"])
                            S.barrier()
                        hTg = sb("hTgF", [128, DC, 512], BF16, ph)
                        nt = NormT(ph, "nF")
                        oxT = sb("oxT", [128, 4, 512], BF16, ph)
                        qxr = Ring(nc, ph, "qxF", [128, 512], BF16, 2)
                        PT = Ring(nc, ph, "PTF", [128, 512], BF16, 4)
                        rr = Ring(nc, ph, "rrF", [128, 512], F32, 2)
                        pf = Ring(nc, ph, "pF", [128, 512], F32, 6, psum=True)
                        tail = Tail(ph, "tF")
                        xs_scale = 128 ** -0.5
                        for tg in range(NG):
                            for ii in range(4):
                                i = tg * 4 + ii
                                nt.run(out[q][i * 128:(i + 1) * 128, :], "gpreF", gpre, hTg, "hTgF", ii * 128)
                            for h in range(4):
                                pq, pqn = pf.next()
                                for c in range(DC):
                                    S.op("pe", lambda e: e.matmul(pq[:], wq[:, c, h * 128:(h + 1) * 128], hTg[:, c, :], start=(c == 0), stop=(c == DC - 1)),
                                         r=["wqF", "hTgF"], w=[pqn])
                                qx_, qxn = qxr.next()
                                S.op("act", lambda e: e.copy(qx_[:], pq[:]), r=[pqn], w=[qxn])
                                pts = []
                                for mt in range(MT):
                                    pst, pstn = pf.next()
                                    S.op("pe", lambda e: e.matmul(pst[:], kxT[:, h, mt * 128:(mt + 1) * 128], qx_[:], start=True, stop=True), r=["kxT", qxn], w=[pstn])
                                    pt_, ptn = PT.next()
                                    S.op("act", lambda e: e.activation(pt_[:], pst[:], AF.Exp, scale=xs_scale), r=[pstn], w=[ptn])
                                    pts.append((pt_, ptn))
                                po, pon = pf.next()
                                pd, pdn = pf.next()
                                for mt in range(MT):
                                    S.op("pe", lambda e: e.matmul(po[:], vx[:, mt, h * 128:(h + 1) * 128], pts[mt][0][:], start=(mt == 0), stop=(mt == MT - 1)),
                                         r=["vx", pts[mt][1]], w=[pon])
                                for mt in range(MT):
                                    S.op("pe", lambda e: e.matmul(pd[:], ones[:], pts[mt][0][:], start=(mt == 0), stop=(mt == MT - 1)),
                                         r=["ones", pts[mt][1]], w=[pdn])
                                r_, rn = rr.next()
                                S.op("dve", lambda e: e.reciprocal(r_[:], pd[:]), r=[pdn], w=[rn])
                                S.op("dve", lambda e: e.tensor_tensor(oxT[:, h, :], po[:], r_[:], ALU.mult), r=[pon, rn], w=["oxT%d" % h])
                            for ii in range(4):
                                i = tg * 4 + ii
                                bk = [pf.next() for _ in range(4)]
                                for cb in range(4):
                                    for hc in range(4):
                                        S.op("pe", lambda e: e.matmul(bk[cb][0][:], oxT[:, hc, ii * 128:(ii + 1) * 128], wo[:, hc, cb * 512:(cb + 1) * 512],
                                                                       start=(hc == 0), stop=(hc == 3)), r=["oxT%d" % hc, "woF"], w=[bk[cb][1]])
                                tail.run([b_[0][:] for b_ in bk], [b_[1] for b_ in bk], "gpostF", gpost,
                                         out[q][i * 128:(i + 1) * 128, :], out[q][i * 128:(i + 1) * 128, :])
                        S.barrier()

                if on("G"):
                    with ExitStack() as ph:
                        gt = sb("gG", [128, D], F32, ph)
                        GT, TPG = 512, 4
                        hTg = sb("hTgG", [128, DC, GT], BF16, ph)
                        aT = sb("aT", [128, 64, GT], BF16, ph)
                        raw = [sb("rawG%d" % i, [128, D], F32, ph) for i in range(TPG)]
                        nt = NormT(ph, "nG")
                        wu = Ring(nc, ph, "wuG", [128, DC, 256], BF16, 2)
                        wd = Ring(nc, ph, "wdG", [128, 4, 512], BF16, 3)
                        pup = Ring(nc, ph, "pGu", [128, 512], F32, 2, psum=True)
                        pdn = [ps("pGd%d" % i, [128, 512], F32, ph) for i in range(TPG)]
                        rl = Ring(nc, ph, "rlG", [128, GT], F32, 2)
                        ssq = sb("ssqG", [128, 32], F32, ph)
                        junk = sb("junkG", [128, 512], BF16, ph)
                        for tg in range(S_ // GT):
                            S.dma("sp", gt[:], A["g_mlp_pre"][l].partition_broadcast(128), "gG", w=["gG"])
                            for ii in range(TPG):
                                i = tg * TPG + ii
                                nt.run(out[q][i * 128:(i + 1) * 128, :], "gG", gt, hTg, "hTgG", ii * 128)
                            S.dma("sp", gt[:], A["g_mlp_post"][l].partition_broadcast(128), "gG", w=["gG"])
                            for b in range(32 if cfg.gcut >= 2 else 0):
                                w_, wn = wu.next()
                                S.dma("sp", w_[:], Wb["up", l][b], wn, r=["w_up%d" % l], w=[wn])
                                for f2 in range(2):
                                    f = b * 2 + f2
                                    p_, pn = pup.next()
                                    for c in range(DC):
                                        S.op("pe", lambda e: e.matmul(p_[:, 0:GT], w_[:, c, f2 * 128:(f2 + 1) * 128], hTg[:, c, :], start=(c == 0), stop=(c == DC - 1)),
                                             r=["hTgG", wn], w=[pn])
                                    r_, rn = rl.next()
                                    S.op("dve", lambda e: e.tensor_scalar(r_[:], p_[:, 0:GT], 0.0, None, ALU.max), r=[pn], w=[rn])
                                    S.op("act", lambda e: e.activation(aT[:, f, :], r_[:], AF.Square), r=[rn], w=["aT%d" % f])
                            for cq4 in range(4 if cfg.gcut >= 3 else 0):
                                for f4 in range(16):
                                    w_, wn = wd.next()
                                    S.dma("sp", w_[:], Wb["down", l][f4 * 512:(f4 + 1) * 512, cq4 * 512:(cq4 + 1) * 512].rearrange("(f p) n -> p f n", p=128),
                                          wn, r=["w_down%d" % l], w=[wn])
                                    for fi in range(4):
                                        f = f4 * 4 + fi
                                        for ii in range(TPG):
                                            S.op("pe", lambda e: e.matmul(pdn[ii][:], aT[:, f, ii * 128:(ii + 1) * 128], w_[:, fi, :], start=(f == 0), stop=(f == 63)),
                                                 r=["aT%d" % f, wn], w=["pGd%d" % ii])
                                for ii in range(TPG if cfg.gcut >= 4 else 0):
                                    S.op("act", lambda e: e.activation(junk[:], pdn[ii][:], AF.Square, accum_out=ssq[:, ii * 8 + cq4:ii * 8 + cq4 + 1]),
                                         r=["pGd%d" % ii], w=["junkG", "ssqG%d" % ii, "pGdx%d" % ii])
                                    S.op("dve", lambda e: e.tensor_copy(raw[ii][:, cq4 * 512:(cq4 + 1) * 512], pdn[ii][:]),
                                         r=["pGd%d" % ii, "pGdx%d" % ii], w=["rawG%d" % ii])
                            for ii in range(TPG if cfg.gcut >= 5 else 0):
                                i = tg * TPG + ii
                                sn = "ssqG%d" % ii
                                xs, xn = nt.xin.next()
                                S.dma("sp", xs[:], out[q][i * 128:(i + 1) * 128, :], xn, w=[xn])
                                S.op("dve", lambda e: e.tensor_reduce(ssq[:, ii * 8 + 4:ii * 8 + 5], ssq[:, ii * 8:ii * 8 + 4], AX.X, ALU.add), r=[sn], w=[sn])
                                S.op("act", lambda e: e.activation(ssq[:, ii * 8 + 5:ii * 8 + 6], ssq[:, ii * 8 + 4:ii * 8 + 5], AF.Sqrt, bias=eps_ap, scale=1.0 / D), r=[sn, "cvec"], w=[sn])
                                S.op("dve", lambda e: e.reciprocal(ssq[:, ii * 8 + 5:ii * 8 + 6], ssq[:, ii * 8 + 5:ii * 8 + 6]), r=[sn], w=[sn])
                                S.op("dve", lambda e: e.tensor_scalar(raw[ii][:], raw[ii][:], ssq[:, ii * 8 + 5:ii * 8 + 6], None, ALU.mult),
                                     r=[sn, "rawG%d" % ii], w=["rawG%d" % ii])
                                S.op("dve", lambda e: e.tensor_tensor(raw[ii][:], raw[ii][:], gt[:], ALU.mult),
                                     r=["gG", "rawG%d" % ii], w=["rawG%d" % ii])
                                S.op("pool", lambda e: e.tensor_tensor(raw[ii][:], raw[ii][:], xs[:], ALU.add), r=[xn, "rawG%d" % ii], w=["rawG%d" % ii])
                                S.dma("pool", out[q][i * 128:(i + 1) * 128, :], raw[ii][:], "rawG%d" % ii, r=["rawG%d" % ii])
                        S.barrier()

        S.barrier()
    return nc


def kernel(**inputs):
    n_cores = 8
    x = np.ascontiguousarray(np.asarray(inputs["x"], dtype=np.float32))
    B, S_, _ = x.shape
    nseq = B // n_cores
    mem = np.ascontiguousarray(np.asarray(inputs["mem"], dtype=np.float32))
    pos = np.ascontiguousarray(np.asarray(inputs["positions"]).astype(np.int32))
    depth = int(np.asarray(inputs["w_in"]).shape[0])
    cfg = Cfg(S=S_, NSEQ=nseq, DEPTH=depth, MEM=mem.shape[1])
    nc = build(cfg)
    consts = host_consts()
    shared = {}
    for name, _ in PARAM_SPECS:
        shared[name] = np.ascontiguousarray(np.asarray(inputs[name], dtype=np.float32))
    for k, v in consts.items():
        shared["c_" + k] = v
    in_maps = []
    for c in range(n_cores):
        m = dict(shared)
        m["x"] = x[c * nseq:(c + 1) * nseq]
        m["mem"] = mem[c * nseq:(c + 1) * nseq]
        m["positions"] = pos[c * nseq:(c + 1) * nseq]
        in_maps.append(m)
    res = run_bass_kernel_spmd(nc, in_maps, core_ids=list(range(n_cores)))
    return np.concatenate([np.asarray(r["out"], dtype=np.float32) for r in res.results], axis=0)
```

```python
import math
from contextlib import ExitStack

import numpy as np
import ml_dtypes

import concourse.bass as bass
import concourse.mybir as mybir
from concourse.bass_utils import run_bass_kernel_spmd

F32 = mybir.dt.float32
BF16 = mybir.dt.bfloat16
I32 = mybir.dt.int32
ALU = mybir.AluOpType
AF = mybir.ActivationFunctionType
AX = mybir.AxisListType

D = 2048
DC = 16
DFF = 8192
INW = 5632
EPS = 1e-6
TWO_PI = 2.0 * math.pi


class Cfg:
    def __init__(self, S=2048, NSEQ=2, DEPTH=2, MEM=256, debug=False, phases=None):
        self.S, self.NSEQ, self.DEPTH, self.MEM, self.debug = S, NSEQ, DEPTH, MEM, debug
        self.NT = S // 128
        self.NG = S // 512
        self.phases = phases
        import os
        self.bcut = int(os.environ.get('BCUT', 9))
        self.gcut = int(os.environ.get('GCUT', 9))


SMALL = {"cvec", "dsc", "s5s", "s5cr", "s5dsk", "tb", "rc_zeta", "rc_gch"}
SMALL_PFX = ("s5ini", "ssqG")
SMALL_SFX = ("_ss0", "_ss1", "_tss0", "_tss1")


class Sch:
    def __init__(self, nc, st):
        self.nc = nc
        self.E = dict(pe=nc.tensor, act=nc.scalar, dve=nc.vector, pool=nc.gpsimd, sp=nc.sync)
        self.sem = {k: st.enter_context(nc.semaphore("e_" + k)) for k in self.E}
        self.cnt = {k: 0 for k in self.E}
        self.pool_sems = [st.enter_context(nc.semaphore("d%d" % i)) for i in range(88)]
        self.pool_cnt = [0] * len(self.pool_sems)
        self.free = list(range(len(self.pool_sems)))
        self.kmap = {}
        self.persist = set()
        self.seen = {k: {} for k in self.E}
        self.lastw = {}
        self.readers = {}

    def _semof(self, key):
        return self.sem[key[1]] if key[0] == "E" else self.pool_sems[key[1]]

    def _wait(self, eng, toks):
        for key, val, small in toks:
            if key == ("E", "pe") and eng == "pe":
                continue
            if self.seen[eng].get(key, 0) >= val:
                continue
            self.E[eng].wait_ge(self._semof(key), val)
            self.seen[eng][key] = val

    def is_small(self, x):
        return x in SMALL or any(x.startswith(p) for p in SMALL_PFX) or x.endswith(SMALL_SFX)

    def _deps(self, r, w):
        toks = []
        for x in r:
            sm = self.is_small(x)
            if x in self.lastw:
                toks.append(self.lastw[x] + (sm,))
        for x in w:
            sm = self.is_small(x)
            if x in self.lastw:
                toks.append(self.lastw[x] + (sm,))
            toks.extend((k, v, sm) for k, v in self.readers.get(x, {}).items())
        return toks

    def _record(self, tok, r, w):
        for x in r:
            d = self.readers.setdefault(x, {})
            d[tok[0]] = max(d.get(tok[0], 0), tok[1])
        for x in w:
            self.lastw[x] = tok
            self.readers[x] = {}

    def op(self, eng, fn, r=(), w=()):
        self._wait(eng, self._deps(r, w))
        ins = fn(self.E[eng])
        self.cnt[eng] += 1
        ins.then_inc(self.sem[eng], 1)
        self._record((("E", eng), self.cnt[eng]), r, w)

    def dkey(self, name, persist=False):
        if name not in self.kmap:
            self.kmap[name] = self.free.pop(0)
            if persist:
                self.persist.add(name)
        return self.kmap[name]

    def dma(self, q, out, in_, key, r=(), w=(), persist=False, **kw):
        self._wait(q, self._deps(r, w))
        ki = self.dkey(key, persist)
        ins = self.E[q].dma_start(out=out, in_=in_, **kw)
        self.pool_cnt[ki] += 16
        ins.then_inc(self.pool_sems[ki], 16)
        self._record((("D", ki), self.pool_cnt[ki]), r, w)

    def barrier(self):
        toks = [(("E", e), self.cnt[e], True) for e in self.E if self.cnt[e] > 0]
        toks += [(("D", i), c, True) for i, c in enumerate(self.pool_cnt) if c > 0]
        for e in self.E:
            self._wait(e, toks)
        self.lastw.clear()
        self.readers.clear()
        for name in list(self.kmap):
            if name not in self.persist:
                self.free.append(self.kmap.pop(name))

    def release_key(self, name):
        self.persist.discard(name)


_UNIQ = [0]


class Ring:
    def __init__(self, nc, st, name, shape, dtype, n, psum=False):
        alloc = nc.psum_tensor if psum else nc.sbuf_tensor
        _UNIQ[0] += 1
        self.t = [st.enter_context(alloc("%s%d_u%d" % (name, i, _UNIQ[0]), shape, dtype)) for i in range(n)]
        self.names = ["%s%d" % (name, i) for i in range(n)]
        self.i = -1

    def next(self):
        self.i = (self.i + 1) % len(self.t)
        return self.t[self.i], self.names[self.i]


class VRing:
    def __init__(self, views, names):
        self.t, self.names, self.i = views, names, -1

    def next(self):
        self.i = (self.i + 1) % len(self.t)
        return self.t[self.i], self.names[self.i]


def t5_bucket(n):
    n = max(n, 0)
    if n < 16:
        return n
    v = 16 + int(np.float32(np.log(np.float32(max(n, 1)) / np.float32(16.0))) / np.float32(math.log(128 / 16)) * np.float32(16))
    return min(v, 31)


def host_consts():
    c = {}
    c["ident"] = np.eye(128, dtype=np.float32).astype(ml_dtypes.bfloat16)
    c["ones"] = np.ones((128, 128), np.float32).astype(ml_dtypes.bfloat16)
    p = np.arange(128)
    cv = np.zeros((128, 8), np.float32)
    cv[:, 0] = 10000.0 ** (-(p % 64) / 64.0)
    cv[:, 1] = np.where(p < 64, -1.0, 1.0)
    cv[:, 2] = (p < 64).astype(np.float32)
    cv[:, 3] = (p >= 64).astype(np.float32)
    cv[:, 4] = EPS
    cv[:, 5] = math.pi / 2
    c["cvec"] = cv
    H, C = 4, 128
    log_g = np.log(1.0 - 2.0 ** (-5.0 - np.arange(H, dtype=np.float64)))
    idx = np.arange(C, dtype=np.float64)
    sc = 128 ** -0.5
    dec = np.zeros((128, H, 128), np.float32)
    for h in range(H):
        dist = idx[None, :] - idx[:, None]
        dec[:, h, :] = np.where(dist >= 0, np.exp(np.maximum(dist, 0) * log_g[h]), 0.0) * sc
    c["decay"] = dec
    xi = np.zeros((128, H, 128), np.float32)
    zt = np.zeros((128, H), np.float32)
    gc = np.zeros((128, H), np.float32)
    for h in range(H):
        xi[:, h, :] = np.exp((idx + 1.0) * log_g[h])[None, :]
        zt[:, h] = np.exp((C - 1.0 - idx) * log_g[h]) * sc
        gc[:, h] = np.exp(C * log_g[h])
    c["xi"] = xi
    c["zeta"] = zt
    c["gch"] = gc
    mk = np.zeros((128, 4, 128), np.float32)
    for kk in range(4):
        for g2 in range(2):
            mk[g2 * 64:(g2 + 1) * 64, kk, 32 * kk + 16 * g2: 32 * kk + 16 * g2 + 16] = 1.0
    c["s5mask"] = mk
    c["iota512"] = np.broadcast_to(np.arange(512, dtype=np.float32)[None, :], (128, 512)).copy()
    oh = np.zeros((2, 128, 32, 128), np.float32)
    cm = np.zeros((128, 128), np.float32)
    for s in range(128):
        for t in range(128):
            if t >= s:
                oh[0, s, t5_bucket(t - s), t] = 1.0
            else:
                cm[s, t] = -200.0
            oh[1, s, t5_bucket(128 + t - s), t] = 1.0
    c["oh"] = oh
    c["cmask"] = cm
    return c


CONST_SPECS = [("ident", [128, 128], BF16), ("ones", [128, 128], BF16), ("cvec", [128, 8], F32),
               ("decay", [128, 4, 128], F32), ("xi", [128, 4, 128], F32), ("zeta", [128, 4], F32),
               ("gch", [128, 4], F32), ("s5mask", [128, 4, 128], F32), ("iota512", [128, 512], F32),
               ("oh", [2, 128, 32, 128], F32), ("cmask", [128, 128], F32)]

PARAM_SPECS = [
    ("rel_bias", [32, 8]), ("w_in", [None, D, INW]), ("w_out", [None, D, D]), ("lam_re", [None, 32, 64]),
    ("lam_im", [None, 32, 64]), ("log_dt", [None, 32]), ("b_re", [None, 32, 64, 16]), ("b_im", [None, 32, 64, 16]),
    ("c_re", [None, 32, 16, 64]), ("c_im", [None, 32, 16, 64]), ("d_skip", [None, 512]), ("w_glu", [None, 512, 512]),
    ("lam_q1", [None, 64]), ("lam_k1", [None, 64]), ("lam_q2", [None, 64]), ("lam_k2", [None, 64]),
    ("g_subln", [None, 128]), ("w_xq", [None, D, 512]), ("w_xkv", [None, D, 1024]), ("w_xo", [None, 512, D]),
    ("w_up", [None, D, DFF]), ("w_down", [None, DFF, D]), ("g_mix_pre", [None, D]), ("g_mix_post", [None, D]),
    ("g_mem", [None, D]), ("g_x_pre", [None, D]), ("g_x_post", [None, D]), ("g_mlp_pre", [None, D]),
    ("g_mlp_post", [None, D]),
]


def build(cfg):
    nc = bass.Bass("TRN2", target_bir_lowering=False)
    S_, NT, NG, NSEQ, DEPTH, MEM = cfg.S, cfg.NT, cfg.NG, cfg.NSEQ, cfg.DEPTH, cfg.MEM
    MT = MEM // 128
    A = {}
    A["x"] = nc.dram_tensor("x", [NSEQ, S_, D], F32, kind="ExternalInput").ap()
    A["mem"] = nc.dram_tensor("mem", [NSEQ, MEM, D], F32, kind="ExternalInput").ap()
    A["positions"] = nc.dram_tensor("positions", [NSEQ, S_], I32, kind="ExternalInput").ap()
    for name, shp in PARAM_SPECS:
        shp = [DEPTH if v is None else v for v in shp]
        A[name] = nc.dram_tensor(name, shp, F32, kind="ExternalInput").ap()
    for name, shp, dt in CONST_SPECS:
        A["c_" + name] = nc.dram_tensor("c_" + name, shp, dt, kind="ExternalInput").ap()
    out = nc.dram_tensor("out", [NSEQ, S_, D], F32, kind="ExternalOutput").ap()
    skind = "ExternalOutput" if cfg.debug else "Internal"

    def scratch(name, shape, dt=BF16):
        return nc.dram_tensor(name, shape, dt, kind=skind).ap()

    Wb = {}
    for l in range(DEPTH):
        Wb["in", l] = scratch("wb_in%d" % l, [11, 128, 16, 512])
        Wb["insw", l] = scratch("wb_insw%d" % l, [2, 128, 16, 512])
        Wb["out", l] = scratch("wb_out%d" % l, [4, 128, 16, 512])
        Wb["xq", l] = scratch("wb_xq%d" % l, [128, 16, 512])
        Wb["xkv", l] = scratch("wb_xkv%d" % l, [2, 128, 16, 512])
        Wb["xo", l] = scratch("wb_xo%d" % l, [128, 4, D])
        Wb["up", l] = scratch("wb_up%d" % l, [32, 128, 16, 256])
        Wb["down", l] = scratch("wb_down%d" % l, [DFF, D])
        Wb["glu", l] = scratch("wb_glu%d" % l, [128, 4, 512])
    fmT = scratch("fmT", [32, 128, S_])
    rv = scratch("rv", [S_, 512])
    dv = scratch("dv", [S_, 1024])
    yT = scratch("yT", [16, 128, S_])

    def on(ph):
        return cfg.phases is None or ph in cfg.phases

    with ExitStack() as st:
        S = Sch(nc, st)

        def sb(name, shape, dt=F32, stack=st):
            _UNIQ[0] += 1
            return stack.enter_context(nc.sbuf_tensor("%s_u%d" % (name, _UNIQ[0]), shape, dt))

        def ps(name, shape, dt=F32, stack=st):
            _UNIQ[0] += 1
            return stack.enter_context(nc.psum_tensor("%s_u%d" % (name, _UNIQ[0]), shape, dt))

        ident = sb("ident", [128, 128], BF16)
        ones = sb("ones", [128, 128], BF16)
        cvec = sb("cvec", [128, 8])
        Bm = sb("Bm", [128, 8, 2, 128])
        tb = sb("tb", [128, 256])
        S.dma("sp", ident[:], A["c_ident"], "ident", w=["ident"])
        S.dma("sp", ones[:], A["c_ones"], "ones", w=["ones"])
        S.dma("sp", cvec[:], A["c_cvec"], "cvec", w=["cvec"])
        S.dma("sp", tb[:], A["rel_bias"].rearrange("b h -> (b h)").partition_broadcast(128), "tb", w=["tb"])
        inv_ap, sgn_ap, m0_ap, m1_ap, eps_ap, hpi_ap = (cvec[:, i:i + 1] for i in range(6))

        def prepass(l):
            k = "w_in%d" % l
            for b in range(11):
                S.dma("pool", Wb["in", l][b], A["w_in"][l][:, b * 512:(b + 1) * 512].rearrange("(c p) n -> p c n", p=128),
                      k, w=[k], persist=True)
            k = "w_insw%d" % l
            for b in range(2):
                src = A["w_in"][l][:, b * 512:(b + 1) * 512].rearrange("(c p) (h two j) -> p c h two j", p=128, two=2, j=64)
                dst = Wb["insw", l][b].rearrange("p c (h two j) -> p c h two j", two=2, j=64)
                for c4 in range(16):
                    S.dma("pool", dst[:, c4, :, 0, :], src[:, c4, :, 1, :], k, w=[k], persist=True)
                    S.dma("pool", dst[:, c4, :, 1, :], src[:, c4, :, 0, :], k, w=[k], persist=True)
            k = "w_out%d" % l
            for b in range(4):
                S.dma("pool", Wb["out", l][b], A["w_out"][l][:, b * 512:(b + 1) * 512].rearrange("(c p) n -> p c n", p=128),
                      k, w=[k], persist=True)
            k = "w_x%d" % l
            S.dma("pool", Wb["xq", l], A["w_xq"][l].rearrange("(c p) n -> p c n", p=128), k, w=[k], persist=True)
            for b in range(2):
                S.dma("pool", Wb["xkv", l][b], A["w_xkv"][l][:, b * 512:(b + 1) * 512].rearrange("(c p) n -> p c n", p=128),
                      k, w=[k], persist=True)
            S.dma("pool", Wb["xo", l], A["w_xo"][l].rearrange("(c p) n -> p c n", p=128), k, w=[k], persist=True)
            S.dma("pool", Wb["glu", l], A["w_glu"][l].rearrange("(c p) n -> p c n", p=128), k, w=[k], persist=True)
            k = "w_up%d" % l
            for b in range(32):
                S.dma("pool", Wb["up", l][b], A["w_up"][l][:, b * 256:(b + 1) * 256].rearrange("(c p) n -> p c n", p=128),
                      k, w=[k], persist=True)
            k = "w_down%d" % l
            for b in range(8):
                S.dma("pool", Wb["down", l][b * 1024:(b + 1) * 1024, :], A["w_down"][l][b * 1024:(b + 1) * 1024, :],
                      k, w=[k], persist=True)

        prepass(0)

        def sincos_tmp(stk, pre, shape):
            return (sb(pre + "_kf", shape, F32, stk), sb(pre + "_ki", shape, I32, stk), sb(pre + "_rr", shape, F32, stk), pre)

        def sincos(tmp, ang, out_s, out_c, rw):
            kf, ki, rr, pre = tmp
            nm = [pre + "_tmp"]
            for which, dst in (("s", out_s), ("c", out_c)):
                if dst is None:
                    continue
                if which == "c":
                    S.op("dve", lambda e: e.tensor_scalar(rr[:], ang, math.pi / 2, None, ALU.add), r=rw, w=nm)
                    src = rr[:]
                else:
                    src = ang
                S.op("dve", lambda e: e.tensor_scalar(kf[:], src, 1.0 / TWO_PI, None, ALU.mult), r=rw + nm, w=nm)
                S.op("dve", lambda e: e.tensor_copy(ki[:], kf[:]), r=nm, w=nm)
                S.op("dve", lambda e: e.tensor_copy(kf[:], ki[:]), r=nm, w=nm)
                S.op("dve", lambda e: e.scalar_tensor_tensor(rr[:], kf[:], -TWO_PI, src, ALU.mult, ALU.add), r=rw + nm, w=nm)
                S.op("dve", lambda e: e.tensor_scalar(kf[:], rr[:], math.pi, -TWO_PI, ALU.is_gt, ALU.mult), r=nm, w=nm)
                S.op("dve", lambda e: e.tensor_tensor(rr[:], rr[:], kf[:], ALU.add), r=nm, w=nm)
                S.op("dve", lambda e: e.tensor_scalar(kf[:], rr[:], -math.pi, TWO_PI, ALU.is_lt, ALU.mult), r=nm, w=nm)
                S.op("dve", lambda e: e.tensor_tensor(rr[:], rr[:], kf[:], ALU.add), r=nm, w=nm)
                S.op("act", lambda e: e.activation(dst, rr[:], AF.Sin), r=nm, w=rw + nm)

        def load_gamma(stk, name, vec_ap):
            t = sb(name, [128, D], F32, stk)
            S.dma("sp", t[:], vec_ap.partition_broadcast(128), name, w=[name])
            return t

        class NormT:
            def __init__(self, stk, pre):
                self.xin = Ring(nc, stk, pre + "_x", [128, D], F32, 2)
                self.hb = Ring(nc, stk, pre + "_hb", [128, D], BF16, 2)
                self.ss = Ring(nc, stk, pre + "_ss", [128, 2], F32, 2)
                self.pt = Ring(nc, stk, pre + "_pt", [128, 8, 128], BF16, 2, psum=True)
                self.flip = 0

            def run(self, src_ap, gname, gt, hT, hres, col0):
                xs, xn = self.xin.next()
                hb, hn = self.hb.next()
                ss, sn = self.ss.next()
                S.dma("sp", xs[:], src_ap, xn, w=[xn])
                S.op("act", lambda e: e.activation(hb[:], xs[:], AF.Square, accum_out=ss[:, 0:1]), r=[xn], w=[hn, sn])
                S.op("act", lambda e: e.activation(ss[:, 1:2], ss[:, 0:1], AF.Sqrt, bias=eps_ap, scale=1.0 / D), r=[sn, "cvec"], w=[sn])
                S.op("dve", lambda e: e.reciprocal(ss[:, 1:2], ss[:, 1:2]), r=[sn], w=[sn])
                S.op("dve", lambda e: e.scalar_tensor_tensor(hb[:], xs[:], ss[:, 1:2], gt[:], ALU.mult, ALU.mult),
                     r=[xn, sn, gname], w=[hn])
                for half in range(2):
                    pt, pn = self.pt.next()
                    for c in range(8):
                        cc = half * 8 + c
                        S.op("pe", lambda e: e.transpose(pt[:, c, :], hb[:, cc * 128:(cc + 1) * 128], ident[:]),
                             r=[hn, "ident"], w=[pn])
                    eng = "act" if self.flip else "dve"
                    self.flip ^= 1
                    dst = hT[:, half * 8:half * 8 + 8, col0:col0 + 128]
                    if eng == "act":
                        S.op("act", lambda e: e.copy(dst, pt[:]), r=[pn], w=[hres])
                    else:
                        S.op("dve", lambda e: e.tensor_copy(dst, pt[:]), r=[pn], w=[hres])

        class Tail:
            def __init__(self, stk, pre):
                self.x = Ring(nc, stk, pre + "_tx", [128, D], F32, 2)
                self.t = Ring(nc, stk, pre + "_tt", [128, D], F32, 2)
                self.ss = Ring(nc, stk, pre + "_tss", [128, 8], F32, 2)
                self.junk = sb(pre + "_junk", [128, 512], BF16, stk)
                self.jn = pre + "_junk"

            def run(self, banks, bnames, gname, gt, x_ap, out_ap):
                xs, xn = self.x.next()
                tt, tn = self.t.next()
                ss, sn = self.ss.next()
                S.dma("sp", xs[:], x_ap, xn, w=[xn])
                for cb in range(4):
                    S.op("act", lambda e: e.activation(self.junk[:], banks[cb], AF.Square, accum_out=ss[:, cb:cb + 1]),
                         r=[bnames[cb]], w=[self.jn, sn])
                S.op("dve", lambda e: e.tensor_reduce(ss[:, 4:5], ss[:, 0:4], AX.X, ALU.add), r=[sn], w=[sn])
                S.op("act", lambda e: e.activation(ss[:, 5:6], ss[:, 4:5], AF.Sqrt, bias=eps_ap, scale=1.0 / D), r=[sn, "cvec"], w=[sn])
                S.op("dve", lambda e: e.reciprocal(ss[:, 5:6], ss[:, 5:6]), r=[sn], w=[sn])
                for cb in range(4):
                    cs = slice(cb * 512, (cb + 1) * 512)
                    S.op("dve", lambda e: e.scalar_tensor_tensor(tt[:, cs], banks[cb], ss[:, 5:6], gt[:, cs], ALU.mult, ALU.mult),
                         r=[bnames[cb], sn, gname], w=[tn])
                S.op("pool", lambda e: e.tensor_tensor(tt[:], tt[:], xs[:], ALU.add), r=[xn, tn], w=[tn])
                S.dma("pool", out_ap, tt[:], tn, r=[tn])

        if on("bias"):
            with ExitStack() as ph:
                oh = sb("oh", [128, 32, 128], F32, ph)
                cm = sb("cm", [128, 128], F32, ph)
                S.dma("sp", cm[:], A["c_cmask"], "cm", w=["cm"])
                for which in range(2):
                    S.dma("sp", oh[:], A["c_oh"][which], "oh", w=["oh"])
                    for h in range(8):
                        eng = "dve"
                        dst = Bm[:, h, which, :]
                        rn = "Bm%d_%d" % (h, which)
                        if which == 0:
                            S.op(eng, lambda e: e.tensor_copy(dst, cm[:]), r=["cm"], w=[rn])
                        else:
                            S.op(eng, lambda e: e.memset(dst, 0.0), w=[rn])
                        for b in range(32):
                            S.op(eng, lambda e: e.scalar_tensor_tensor(dst, oh[:, b, :], tb[:, b * 8 + h:b * 8 + h + 1], dst,
                                                                        ALU.mult, ALU.add), r=["oh", "tb", rn], w=[rn])
                S.barrier()

        for l in range(DEPTH):
            lam_init = 0.8 - 0.6 * math.exp(-0.3 * l)
            for q in range(NSEQ):
                xsrc = A["x"][q] if l == 0 else out[q]

                if on("A"):
                    with ExitStack() as ph:
                        hT = sb("hT", [128, DC, S_], BF16, ph)
                        hres = ["hT%d" % i for i in range(NT)]
                        with ExitStack() as ph1:
                            gpre = load_gamma(ph1, "gpre", A["g_mix_pre"][l])
                            nt = NormT(ph1, "nA")
                            for i in range(NT):
                                nt.run(xsrc[i * 128:(i + 1) * 128, :], "gpre", gpre, hT, hres[i], i * 128)
                            S.barrier()
                        cosT = sb("cosT", [128, S_], F32, ph)
                        sinT = sb("sinT", [128, S_], F32, ph)
                        with ExitStack() as ph2:
                            posi = sb("posi", [128, S_], I32, ph2)
                            ang = sb("ang", [128, S_], F32, ph2)
                            S.dma("sp", posi[:], A["positions"][q].partition_broadcast(128), "posi", w=["posi"])
                            S.op("dve", lambda e: e.tensor_copy(ang[:], posi[:]), r=["posi"], w=["ang"])
                            S.op("dve", lambda e: e.tensor_scalar(ang[:], ang[:], inv_ap, None, ALU.mult), r=["ang", "cvec"], w=["ang"])
                            sincos(sincos_tmp(ph2, "rot", [128, S_]), ang[:], sinT[:], cosT[:], ["ang", "sinT", "cosT"])
                            S.op("dve", lambda e: e.tensor_scalar(sinT[:], sinT[:], sgn_ap, None, ALU.mult), r=["sinT", "cvec"], w=["sinT"])
                            S.barrier()
                        wring = Ring(nc, ph, "wA", [128, DC, 512], BF16, 2)
                        wsw = Ring(nc, ph, "wAs", [128, DC, 512], BF16, 1)
                        pacc = Ring(nc, ph, "pA", [128, 512], F32, 4, psum=True)
                        pswp = Ring(nc, ph, "pAs", [128, 512], F32, 2, psum=True)
                        stg = Ring(nc, ph, "stgA", [128, S_], BF16, 3)
                        stgT = Ring(nc, ph, "stgT", [128, 512], BF16, 3)
                        tmp1 = Ring(nc, ph, "tmpA", [128, 512], F32, 2)
                        tmp2 = Ring(nc, ph, "tmpB", [128, 512], F32, 2)
                        blocks = [("rot", 0), ("rot", 4), ("tm", (rv, 0)), ("silu", 8), ("copy", 12), ("copy", 16), ("copy", 20),
                                  ("copy", 24), ("copy", 28), ("tm", (dv, 0)), ("tm", (dv, 512))]
                        for b, (kind, arg) in enumerate(blocks):
                            wt, wn = wring.next()
                            S.dma("sp", wt[:], Wb["in", l][b], wn, r=["w_in%d" % l], w=[wn])
                            if kind == "rot":
                                ws, wsn = wsw.next()
                                S.dma("sp", ws[:], Wb["insw", l][b], wsn, r=["w_insw%d" % l], w=[wsn])
                            if kind == "tm":
                                dst, c0 = arg
                                for i in range(NT):
                                    pa, pn = pacc.next()
                                    for c in range(DC):
                                        S.op("pe", lambda e: e.matmul(pa[:], hT[:, c, i * 128:(i + 1) * 128], wt[:, c, :],
                                                                       start=(c == 0), stop=(c == DC - 1)), r=[hres[i], wn], w=[pn])
                                    sg, sgn = stgT.next()
                                    eng = "act" if i % 2 else "dve"
                                    if eng == "act":
                                        S.op("act", lambda e: e.copy(sg[:], pa[:]), r=[pn], w=[sgn])
                                    else:
                                        S.op("dve", lambda e: e.tensor_copy(sg[:], pa[:]), r=[pn], w=[sgn])
                                    S.dma("pool", dst[i * 128:(i + 1) * 128, c0:c0 + 512], sg[:], sgn, r=[sgn])
                                continue
                            for j in range(4):
                                sg, sgn = stg.next()
                                for tg in range(NG):
                                    ts = slice(tg * 512, (tg + 1) * 512)
                                    hr = hres[tg * 4:(tg + 1) * 4]
                                    pa, pn = pacc.next()
                                    for c in range(DC):
                                        S.op("pe", lambda e: e.matmul(pa[:], wt[:, c, j * 128:(j + 1) * 128], hT[:, c, ts],
                                                                       start=(c == 0), stop=(c == DC - 1)), r=hr + [wn], w=[pn])
                                    if kind == "rot":
                                        pb, pbn = pswp.next()
                                        for c in range(DC):
                                            S.op("pe", lambda e: e.matmul(pb[:], ws[:, c, j * 128:(j + 1) * 128], hT[:, c, ts],
                                                                           start=(c == 0), stop=(c == DC - 1)), r=hr + [wsn], w=[pbn])
                                        t1, t1n = tmp1.next()
                                        t2, t2n = tmp2.next()
                                        S.op("dve", lambda e: e.tensor_tensor(t1[:], pa[:], cosT[:, ts], ALU.mult), r=[pn, "cosT"], w=[t1n])
                                        S.op("dve", lambda e: e.tensor_tensor(t2[:], pb[:], sinT[:, ts], ALU.mult), r=[pbn, "sinT"], w=[t2n])
                                        S.op("pool", lambda e: e.tensor_tensor(sg[:, ts], t1[:], t2[:], ALU.add), r=[t1n, t2n], w=[sgn])
                                    elif kind == "silu":
                                        S.op("act", lambda e: e.activation(sg[:, ts], pa[:], AF.Silu), r=[pn], w=[sgn])
                                    else:
                                        if (j + tg) % 2:
                                            S.op("act", lambda e: e.copy(sg[:, ts], pa[:]), r=[pn], w=[sgn])
                                        else:
                                            S.op("dve", lambda e: e.tensor_copy(sg[:, ts], pa[:]), r=[pn], w=[sgn])
                                S.dma("pool", fmT[arg + j], sg[:], sgn, r=[sgn])
                        S.barrier()

                if on("B"):
                    with ExitStack() as ph:
                        decay = sb("decay", [128, 4, 128], F32, ph)
                        xi = sb("xi", [128, 4, 128], F32, ph)
                        zeta = sb("zeta", [128, 4], F32, ph)
                        gch = sb("gch", [128, 4], F32, ph)
                        for nm_, t_ in (("decay", decay), ("xi", xi), ("zeta", zeta), ("gch", gch)):
                            S.dma("sp", t_[:], A["c_" + nm_], "rc_" + nm_, w=["rc_" + nm_])
                        qT = sb("rqT", [128, 4, S_], BF16, ph)
                        kT = sb("rkT", [128, 4, S_], BF16, ph)
                        gT = sb("rgT", [128, 4, S_], BF16, ph)
                        qx = sb("rqx", [128, 4, S_], BF16, ph)
                        vv = sb("rvv", [128, NT, 512], BF16, ph)
                        yst = sb("ryst", [128, 4, S_], BF16, ph)
                        R32 = sb("R32", [128, 4, 128], F32, ph)
                        Rb = sb("Rb", [128, 4, 128], BF16, ph)
                        for h in range(4):
                            S.dma("sp", qT[:, h, :], fmT[h], "rq%d" % h, w=["rq%d" % h])
                            S.dma("sp", kT[:, h, :], fmT[4 + h], "rk%d" % h, w=["rk%d" % h])
                            S.dma("sp", gT[:, h, :], fmT[8 + h], "rg%d" % h, w=["rg%d" % h])
                        S.dma("sp", vv[:], rv.rearrange("(i p) e -> p i e", p=128), "rvv", w=["rvv"])
                        for h in range(4):
                            S.op("pool", lambda e: e.tensor_tensor(qx[:, h, :].rearrange("p (c t) -> p c t", t=128),
                                                                   qT[:, h, :].rearrange("p (c t) -> p c t", t=128),
                                                                   xi[:, h, :].unsqueeze(1).broadcast_to([128, NT, 128]), ALU.mult),
                                 r=["rq%d" % h, "rc_xi"], w=["rqx%d" % h])
                        pBa = ps("pBa", [128, 4, 128], F32, ph)
                        pBb = ps("pBb", [128, 4, 128], BF16, ph)
                        p_in = VRing([pBa[:, 0, :], pBa[:, 1, :]], ["pBi0", "pBi1"])
                        pBc = ps("pBc", [128, 4, 128], F32, ph)
                        p_dr = VRing([pBc[:, 0, :], pBc[:, 2, :]], ["pBd0", "pBd1"])
                        R32b = sb("R32b", [128, 4, 128], F32, ph)
                        p_kt = VRing([pBb[:, 0, :], pBb[:, 1, :]], ["pBk0", "pBk1"])
                        p_o = [ps("pBo%d" % h, [128, 512], F32, ph) for h in range(4)]
                        p_ss = ps("pBss", [128, 512], F32, ph)
                        msk = Ring(nc, ph, "mskB", [128, 128], BF16, 3)
                        kz = Ring(nc, ph, "kzB", [128, 128], BF16, 2)
                        sq = Ring(nc, ph, "sqB", [128, 512], BF16, 2)
                        rt = Ring(nc, ph, "rtB", [128, 512], F32, 2)
                        tB = Ring(nc, ph, "tB", [128, 512], F32, 2)
                        for c in range(NT if cfg.bcut >= 2 else 0):
                            cs = slice(c * 128, (c + 1) * 128)
                            oc = slice((c % 4) * 128, (c % 4) * 128 + 128)
                            for h in range(4):
                                pi, pin = p_in.next()
                                S.op("pe", lambda e: e.matmul(pi[:], kT[:, h, cs], qT[:, h, cs], start=True, stop=True),
                                     r=["rk%d" % h, "rq%d" % h], w=[pin])
                                mk, mkn = msk.next()
                                S.op("dve", lambda e: e.tensor_tensor(mk[:], pi[:], decay[:, h, :], ALU.mult), r=[pin, "rc_decay"], w=[mkn])
                                S.op("pe", lambda e: e.matmul(p_o[h][:, oc], vv[:, c, h * 128:(h + 1) * 128], mk[:], start=True, stop=(c == 0)),
                                     r=["rvv", mkn], w=["pBo%d" % h])
                                if c > 0 and cfg.bcut >= 3:
                                    S.op("pe", lambda e: e.matmul(p_o[h][:, oc], Rb[:, h, :], qx[:, h, cs], start=False, stop=True),
                                         r=["Rb%d" % h, "rqx%d" % h], w=["pBo%d" % h])
                                if c < NT - 1 and cfg.bcut >= 3:
                                    pk, pkn = p_kt.next()
                                    S.op("pe", lambda e: e.transpose(pk[:], kT[:, h, cs], ident[:]), r=["rk%d" % h, "ident"], w=[pkn])
                                    kzt, kzn = kz.next()
                                    S.op("dve", lambda e: e.tensor_scalar(kzt[:], pk[:], zeta[:, h:h + 1], None, ALU.mult), r=[pkn, "rc_zeta"], w=[kzn])
                                    pd, pdn = p_dr.next()
                                    S.op("pe", lambda e: e.matmul(pd[:], kzt[:], vv[:, c, h * 128:(h + 1) * 128], start=True, stop=True),
                                         r=[kzn, "rvv"], w=[pdn])
                                    Rn = (R32, R32b)[c % 2]
                                    Ro = (R32, R32b)[(c + 1) % 2]
                                    if c == 0:
                                        S.op("dve", lambda e: e.tensor_copy(Rn[:, h, :], pd[:]), r=[pdn], w=["R32%d" % h])
                                    else:
                                        S.op("dve", lambda e: e.tensor_scalar(Rn[:, h, :], Ro[:, h, :], gch[:, h:h + 1], None, ALU.mult),
                                             r=["R32%d" % h, "rc_gch"], w=["R32%d" % h])
                                        S.op("dve", lambda e: e.tensor_tensor(Rn[:, h, :], Rn[:, h, :], pd[:], ALU.add), r=[pdn, "R32%d" % h], w=["R32%d" % h])
                                    S.op("dve", lambda e: e.tensor_copy(Rb[:, h, :], Rn[:, h, :]), r=["R32%d" % h], w=["Rb%d" % h])
                                if c % 4 == 3 and cfg.bcut >= 4:
                                    ts = slice((c - 3) * 128, (c + 1) * 128)
                                    sqt, sqn = sq.next()
                                    rtt, rtn = rt.next()
                                    tt, tn = tB.next()
                                    S.op("act", lambda e: e.activation(sqt[:], p_o[h][:], AF.Square), r=["pBo%d" % h], w=[sqn])
                                    S.op("pe", lambda e: e.matmul(p_ss[:], ones[:], sqt[:], start=True, stop=True), r=[sqn, "ones"], w=["pBss"])
                                    S.op("act", lambda e: e.activation(rtt[:], p_ss[:], AF.Sqrt, bias=eps_ap, scale=1.0 / 128), r=["pBss", "cvec"], w=[rtn])
                                    S.op("dve", lambda e: e.reciprocal(rtt[:], rtt[:]), r=[rtn], w=[rtn])
                                    S.op("dve", lambda e: e.tensor_tensor(tt[:], p_o[h][:], rtt[:], ALU.mult), r=["pBo%d" % h, rtn], w=[tn])
                                    S.op("pool", lambda e: e.tensor_tensor(yst[:, h, ts], tt[:], gT[:, h, ts], ALU.mult),
                                         r=[tn, "rg%d" % h], w=["ryst%d" % h])
                        for h in range(4):
                            S.dma("pool", yT[h], yst[:, h, :], "ryst%d" % h, r=["ryst%d" % h])
                        S.barrier()

                if on("C"):
                    with ExitStack() as ph:
                        LB = [sb("LB%d" % i, [128, 16, 128], BF16, ph) for i in range(2)]
                        LC = [sb("LC%d" % i, [128, 16, 128], BF16, ph) for i in range(2)]
                        th = sb("s5th", [128, 16], F32, ph)
                        mag = sb("s5mag", [128, 16], F32, ph)
                        cL = sb("s5cL", [128, 16], F32, ph)
                        sL = sb("s5sL", [128, 16], F32, ph)
                        dsk = sb("s5dsk", [128, 4], F32, ph)
                        wgl = sb("s5wgl", [128, 4, 512], BF16, ph)
                        S.dma("sp", wgl[:], Wb["glu", l], "s5wgl", r=["w_x%d" % l], w=["s5wgl"])
                        S.dma("sp", dsk[:], A["d_skip"][l].rearrange("(c p) -> p c", p=128), "s5dsk", w=["s5dsk"], allow_slow_non_contiguous=True)
                        with ExitStack() as p1:
                            lr = sb("s5lr", [128, 16], F32, p1)
                            li = sb("s5li", [128, 16], F32, p1)
                            dt = sb("s5dt", [128, 16], F32, p1)
                            S.dma("sp", lr[:], A["lam_re"][l].rearrange("(k g2) p -> (g2 p) k", g2=2), "s5lr", w=["s5s"], allow_slow_non_contiguous=True)
                            S.dma("sp", li[:], A["lam_im"][l].rearrange("(k g2) p -> (g2 p) k", g2=2), "s5li", w=["s5s"], allow_slow_non_contiguous=True)
                            ldv = A["log_dt"][l].rearrange("(k g2) -> g2 k", g2=2)
                            for g2 in range(2):
                                S.dma("sp", dt[g2 * 64:(g2 + 1) * 64, :], ldv[g2:g2 + 1, :].broadcast_to([64, 16]), "s5dt", w=["s5s"],
                                      allow_slow_non_contiguous=True)
                            sm = ["s5s"]
                            t16 = [sb("s5t%d" % i, [128, 16], F32, p1) for i in range(8)]
                            abr, abi, nr, den, fre, fim, u0, u1 = t16
                            V = lambda fn: S.op("dve", fn, r=sm + ["cvec"], w=sm)
                            S.op("act", lambda e: e.activation(dt[:], dt[:], AF.Exp), r=sm, w=sm)
                            V(lambda e: e.tensor_scalar(lr[:], lr[:], -1e-4, None, ALU.min))
                            V(lambda e: e.tensor_tensor(u0[:], lr[:], dt[:], ALU.mult))
                            S.op("act", lambda e: e.activation(mag[:], u0[:], AF.Exp), r=sm, w=sm)
                            V(lambda e: e.tensor_tensor(th[:], li[:], dt[:], ALU.mult))
                            sc16 = sincos_tmp(p1, "s5a", [128, 16])
                            sincos(sc16, th[:], abi[:], abr[:], sm)
                            V(lambda e: e.tensor_tensor(abr[:], abr[:], mag[:], ALU.mult))
                            V(lambda e: e.tensor_tensor(abi[:], abi[:], mag[:], ALU.mult))
                            V(lambda e: e.tensor_scalar(nr[:], abr[:], -1.0, None, ALU.add))
                            V(lambda e: e.tensor_tensor(den[:], lr[:], lr[:], ALU.mult))
                            V(lambda e: e.tensor_tensor(u0[:], li[:], li[:], ALU.mult))
                            V(lambda e: e.tensor_tensor(den[:], den[:], u0[:], ALU.add))
                            V(lambda e: e.reciprocal(den[:], den[:]))
                            V(lambda e: e.tensor_tensor(u0[:], nr[:], lr[:], ALU.mult))
                            V(lambda e: e.tensor_tensor(u1[:], abi[:], li[:], ALU.mult))
                            V(lambda e: e.tensor_tensor(fre[:], u0[:], u1[:], ALU.add))
                            V(lambda e: e.tensor_tensor(fre[:], fre[:], den[:], ALU.mult))
                            V(lambda e: e.tensor_tensor(u0[:], abi[:], lr[:], ALU.mult))
                            V(lambda e: e.tensor_tensor(u1[:], nr[:], li[:], ALU.mult))
                            V(lambda e: e.tensor_tensor(fim[:], u0[:], u1[:], ALU.subtract))
                            V(lambda e: e.tensor_tensor(fim[:], fim[:], den[:], ALU.mult))
                            V(lambda e: e.tensor_scalar(u0[:], th[:], 512.0, None, ALU.mult))
                            sincos(sc16, u0[:], sL[:], cL[:], sm)
                            bre = sb("s5bre", [128, 16, 16], F32, p1)
                            bim = sb("s5bim", [128, 16, 16], F32, p1)
                            S.dma("sp", bre[:], A["b_re"][l].rearrange("(k g2) p h -> (g2 p) k h", g2=2), "s5bre", w=sm)
                            S.dma("sp", bim[:], A["b_im"][l].rearrange("(k g2) p h -> (g2 p) k h", g2=2), "s5bim", w=sm)
                            bbr = sb("s5bbr", [128, 16, 16], F32, p1)
                            bbi = sb("s5bbi", [128, 16, 16], F32, p1)
                            v0 = sb("s5v0", [128, 16, 16], F32, p1)
                            fr3 = fre[:].unsqueeze(2).broadcast_to([128, 16, 16])
                            fi3 = fim[:].unsqueeze(2).broadcast_to([128, 16, 16])
                            V(lambda e: e.tensor_tensor(bbr[:], bre[:], fr3, ALU.mult))
                            V(lambda e: e.tensor_tensor(v0[:], bim[:], fi3, ALU.mult))
                            V(lambda e: e.tensor_tensor(bbr[:], bbr[:], v0[:], ALU.subtract))
                            V(lambda e: e.tensor_tensor(bbi[:], bim[:], fr3, ALU.mult))
                            V(lambda e: e.tensor_tensor(v0[:], bre[:], fi3, ALU.mult))
                            V(lambda e: e.tensor_tensor(bbi[:], bbi[:], v0[:], ALU.add))
                            xpad = sb("s5xpad", [128, 16, 128], BF16, p1)
                            ptp = Ring(nc, p1, "s5ptp", [128, 128], BF16, 2, psum=True)
                            for ri, bb in enumerate((bbr, bbi)):
                                V(lambda e: e.memset(xpad[:], 0.0))
                                for k in range(16):
                                    off = 32 * (k % 4)
                                    V(lambda e: e.tensor_scalar(xpad[:, k, off:off + 16], bb[:, k, :], m0_ap, None, ALU.mult))
                                    V(lambda e: e.tensor_scalar(xpad[:, k, off + 16:off + 32], bb[:, k, :], m1_ap, None, ALU.mult))
                                for k in range(16):
                                    pt_, ptn = ptp.next()
                                    S.op("pe", lambda e: e.transpose(pt_[:], xpad[:, k, :], ident[:]), r=sm + ["ident"], w=[ptn])
                                    S.op("act", lambda e: e.copy(LB[ri][:, k, :], pt_[:]), r=[ptn], w=["LB"])
                            s5mask = sb("s5mask", [128, 4, 128], F32, p1)
                            S.dma("sp", s5mask[:], A["c_s5mask"], "s5mask", w=sm)
                            c32 = sb("s5c32", [128, 4, 64], F32, p1)
                            cdup = sb("s5cdup", [128, 4, 128], BF16, p1)
                            for ri, nm_ in enumerate(("c_re", "c_im")):
                                S.dma("sp", c32[:], A[nm_][l].rearrange("(uc gl) h p -> (gl h) uc p", gl=8), "s5c32", w=sm)
                                V(lambda e: e.tensor_copy(cdup[:, :, 0:64], c32[:]))
                                V(lambda e: e.tensor_copy(cdup[:, :, 64:128], c32[:]))
                                for uc in range(4):
                                    pt_, ptn = ptp.next()
                                    S.op("pe", lambda e: e.transpose(pt_[:], cdup[:, uc, :], ident[:]), r=sm + ["ident"], w=[ptn])
                                    for kk in range(4):
                                        k = uc * 4 + kk
                                        S.op("dve", lambda e: e.scalar_tensor_tensor(LC[ri][:, k, :], pt_[:], (1.0 if ri == 0 else -1.0),
                                                                                     s5mask[:, kk, :], ALU.mult, ALU.mult), r=[ptn] + sm, w=["LC"])
                            S.barrier()
                        uT = sb("s5uT", [128, 4, S_], BF16, ph)
                        gTt = sb("s5gT", [128, 4, S_], BF16, ph)
                        for uc in range(4):
                            S.dma("sp", uT[:, uc, :], fmT[12 + uc], "s5u%d" % uc, w=["s5u%d" % uc])
                        iota = sb("s5iota", [128, 512], F32, ph)
                        S.dma("sp", iota[:], A["c_iota512"], "s5iota", w=["s5iota"])
                        tabC = sb("s5tabC", [128, 4, 512], F32, ph)
                        tabS = sb("s5tabS", [128, 4, 512], F32, ph)
                        rtab = sb("s5rtab", [128, 4, 512], F32, ph)
                        angk = sb("s5angk", [128, 512], F32, ph)
                        ini = sb("s5ini", [128, 4, 2], F32, ph)
                        p_bu = Ring(nc, ph, "pCb", [128, 2, 512], F32, 2, psum=True)
                        p_y = Ring(nc, ph, "pCy", [128, 512], F32, 2, psum=True)
                        p_gl = Ring(nc, ph, "pCg", [128, 512], F32, 2, psum=True)
                        m_t = [Ring(nc, ph, "s5m%d" % i, [128, 512], F32, 2) for i in range(4)]
                        mre = Ring(nc, ph, "s5mre", [128, 512], F32, 2)
                        mim = Ring(nc, ph, "s5mim", [128, 512], F32, 2)
                        zre = Ring(nc, ph, "s5zre", [128, 512], F32, 2)
                        zim = Ring(nc, ph, "s5zim", [128, 512], F32, 2)
                        d_t = [Ring(nc, ph, "s5d%d" % i, [128, 512], F32, 2) for i in range(4)]
                        xre = Ring(nc, ph, "s5xre", [128, 512], BF16, 2)
                        xim = Ring(nc, ph, "s5xim", [128, 512], BF16, 2)
                        yv = Ring(nc, ph, "s5yv", [128, 512], F32, 2)
                        e1 = Ring(nc, ph, "s5e1", [128, 512], F32, 2)
                        e2 = Ring(nc, ph, "s5e2", [128, 512], F32, 2)
                        cr = sb("s5cr", [128, 4], F32, ph)
                        sc512 = sincos_tmp(ph, "s5c", [128, 512])
                        TT = ALU
                        for uc in range(4):
                            for kk in range(4):
                                k = uc * 4 + kk
                                tn_ = "s5tab%d" % kk
                                S.op("dve", lambda e: e.tensor_scalar(angk[:], iota[:], th[:, k:k + 1], None, ALU.mult), r=["s5iota", "s5s"], w=["s5angk"])
                                sincos(sc512, angk[:], tabS[:, kk, :], tabC[:, kk, :], ["s5angk", tn_])
                                S.op("dve", lambda e: e.memset(rtab[:, kk, :], 1.0), w=[tn_ + "r"])
                                S.op("dve", lambda e: e.tensor_scalar(rtab[:, kk, :], rtab[:, kk, :], mag[:, k:k + 1], None, ALU.mult), r=["s5s"], w=[tn_ + "r"])
                            for j in range(NG):
                                ts = slice(j * 512, (j + 1) * 512)
                                py, pyn = p_y.next()
                                for kk in range(4):
                                    k = uc * 4 + kk
                                    tn_ = "s5tab%d" % kk
                                    Ck, Sk = tabC[:, kk, :], tabS[:, kk, :]
                                    pb, pbn = p_bu.next()
                                    for ri in range(2):
                                        S.op("pe", lambda e: e.matmul(pb[:, ri, :], LB[ri][:, k, :], uT[:, uc, ts], start=True, stop=True),
                                             r=["LB", "s5u%d" % uc], w=[pbn])
                                    ms = [r_.next() for r_ in m_t]
                                    (a0, a0n), (a1, a1n), (a2, a2n), (a3, a3n) = ms
                                    mr, mrn = mre.next()
                                    mi, min_ = mim.next()
                                    S.op("dve", lambda e: e.tensor_tensor(a0[:], pb[:, 0, :], Ck, TT.mult), r=[pbn, tn_], w=[a0n])
                                    S.op("dve", lambda e: e.tensor_tensor(a1[:], pb[:, 1, :], Sk, TT.mult), r=[pbn, tn_], w=[a1n])
                                    S.op("pool", lambda e: e.tensor_tensor(mr[:], a0[:], a1[:], TT.add), r=[a0n, a1n], w=[mrn])
                                    S.op("dve", lambda e: e.tensor_tensor(a2[:], pb[:, 1, :], Ck, TT.mult), r=[pbn, tn_], w=[a2n])
                                    S.op("dve", lambda e: e.tensor_tensor(a3[:], pb[:, 0, :], Sk, TT.mult), r=[pbn, tn_], w=[a3n])
                                    S.op("pool", lambda e: e.tensor_tensor(mi[:], a2[:], a3[:], TT.subtract), r=[a2n, a3n], w=[min_])
                                    zr, zrn = zre.next()
                                    zi, zin = zim.next()
                                    inr = 0.0 if j == 0 else ini[:, kk, 0:1]
                                    ini_ = 0.0 if j == 0 else ini[:, kk, 1:2]
                                    inn = "s5ini%d" % kk
                                    S.op("dve", lambda e: e.tensor_tensor_scan(zr[:], rtab[:, kk, :], mr[:], inr, TT.mult, TT.add),
                                         r=[tn_ + "r", mrn, inn], w=[zrn])
                                    S.op("dve", lambda e: e.tensor_tensor_scan(zi[:], rtab[:, kk, :], mi[:], ini_, TT.mult, TT.add),
                                         r=[tn_ + "r", min_, inn], w=[zin])
                                    if j < NG - 1:
                                        S.op("dve", lambda e: e.tensor_tensor(cr[:, 0:1], zi[:, 511:512], sL[:, k:k + 1], TT.mult), r=[zin, "s5s"], w=["s5cr"])
                                        S.op("dve", lambda e: e.scalar_tensor_tensor(ini[:, kk, 0:1], zr[:, 511:512], cL[:, k:k + 1], cr[:, 0:1], TT.mult, TT.subtract),
                                             r=[zrn, "s5cr", "s5s"], w=[inn])
                                        S.op("dve", lambda e: e.tensor_tensor(cr[:, 1:2], zr[:, 511:512], sL[:, k:k + 1], TT.mult), r=[zrn, "s5s"], w=["s5cr"])
                                        S.op("dve", lambda e: e.scalar_tensor_tensor(ini[:, kk, 1:2], zi[:, 511:512], cL[:, k:k + 1], cr[:, 1:2], TT.mult, TT.add),
                                             r=[zin, "s5cr", "s5s"], w=[inn])
                                    ds = [r_.next() for r_ in d_t]
                                    (b0, b0n), (b1, b1n), (b2, b2n), (b3, b3n) = ds
                                    xr, xrn = xre.next()
                                    xi_, xin_ = xim.next()
                                    S.op("pool", lambda e: e.tensor_tensor(b0[:], zr[:], Ck, TT.mult), r=[zrn, tn_], w=[b0n])
                                    S.op("pool", lambda e: e.tensor_tensor(b1[:], zi[:], Sk, TT.mult), r=[zin, tn_], w=[b1n])
                                    S.op("pool", lambda e: e.tensor_tensor(xr[:], b0[:], b1[:], TT.subtract), r=[b0n, b1n], w=[xrn])
                                    S.op("pool", lambda e: e.tensor_tensor(b2[:], zi[:], Ck, TT.mult), r=[zin, tn_], w=[b2n])
                                    S.op("pool", lambda e: e.tensor_tensor(b3[:], zr[:], Sk, TT.mult), r=[zrn, tn_], w=[b3n])
                                    S.op("pool", lambda e: e.tensor_tensor(xi_[:], b2[:], b3[:], TT.add), r=[b2n, b3n], w=[xin_])
                                    S.op("pe", lambda e: e.matmul(py[:], LC[0][:, k, :], xr[:], start=(kk == 0), stop=False), r=["LC", xrn], w=[pyn])
                                    S.op("pe", lambda e: e.matmul(py[:], LC[1][:, k, :], xi_[:], start=False, stop=(kk == 3)), r=["LC", xin_], w=[pyn])
                                yt_, ytn = yv.next()
                                f1, f1n = e1.next()
                                f2, f2n = e2.next()
                                S.op("dve", lambda e: e.scalar_tensor_tensor(yt_[:], uT[:, uc, ts], dsk[:, uc:uc + 1], py[:], TT.mult, TT.add),
                                     r=["s5u%d" % uc, "s5dsk", pyn], w=[ytn])
                                S.op("act", lambda e: e.activation(f1[:], yt_[:], AF.Square), r=[ytn], w=[f1n])
                                S.op("dve", lambda e: e.tensor_scalar(f1[:], f1[:], 0.044715, 1.0, TT.mult, TT.add), r=[f1n], w=[f1n])
                                S.op("dve", lambda e: e.tensor_tensor(f1[:], f1[:], yt_[:], TT.mult), r=[f1n, ytn], w=[f1n])
                                S.op("act", lambda e: e.activation(f2[:], f1[:], AF.Sigmoid, scale=2.0 * math.sqrt(2.0 / math.pi)), r=[f1n], w=[f2n])
                                S.op("pool", lambda e: e.tensor_tensor(gTt[:, uc, ts], yt_[:], f2[:], TT.mult), r=[ytn, f2n], w=["s5g%d_%d" % (uc, j)])
                        yst = sb("s5yst", [128, 4, S_], BF16, ph)
                        sg_ = Ring(nc, ph, "s5sg", [128, 512], F32, 2)
                        for j in range(NG):
                            ts = slice(j * 512, (j + 1) * 512)
                            for oc in range(4):
                                pg, pgn = p_gl.next()
                                for ci in range(4):
                                    S.op("pe", lambda e: e.matmul(pg[:], wgl[:, ci, oc * 128:(oc + 1) * 128], gTt[:, ci, ts], start=(ci == 0), stop=(ci == 3)),
                                         r=["s5wgl", "s5g%d_%d" % (ci, j)], w=[pgn])
                                sgt, sgn = sg_.next()
                                S.op("act", lambda e: e.activation(sgt[:], pg[:], AF.Sigmoid), r=[pgn], w=[sgn])
                                S.op("dve", lambda e: e.tensor_tensor(yst[:, oc, ts], gTt[:, oc, ts], sgt[:], TT.mult), r=[sgn, "s5g%d_%d" % (oc, j)], w=["s5yst%d" % oc])
                        for oc in range(4):
                            S.dma("pool", yT[4 + oc], yst[:, oc, :], "s5yst%d" % oc, r=["s5yst%d" % oc])
                        S.barrier()

                if q == 0 and l + 1 < DEPTH:
                    prepass(l + 1)

                if on("D"):
                    with ExitStack() as ph:
                        scale = 64 ** -0.5
                        lq = sb("dlq", [128, 4, 64], F32, ph)
                        for i_, nm_ in enumerate(("lam_q1", "lam_k1", "lam_q2", "lam_k2")):
                            S.dma("sp", lq[:, i_, :], A[nm_][l].partition_broadcast(128), "dlq", w=["dsc"])
                        sc = sb("dsc", [128, 8], F32, ph)
                        pr = sb("dpr", [128, 2, 64], F32, ph)
                        gs = sb("dgs", [128, 1], F32, ph)
                        S.dma("sp", gs[:], A["g_subln"][l].rearrange("(p o) -> p o", o=1), "dgs", w=["dsc"])
                        V = lambda fn: S.op("dve", fn, r=["dsc"], w=["dsc"])
                        V(lambda e: e.tensor_tensor(pr[:, 0, :], lq[:, 0, :], lq[:, 1, :], ALU.mult))
                        V(lambda e: e.tensor_tensor(pr[:, 1, :], lq[:, 2, :], lq[:, 3, :], ALU.mult))
                        V(lambda e: e.tensor_reduce(sc[:, 0:2], pr[:], AX.X, ALU.add))
                        S.op("act", lambda e: e.activation(sc[:, 0:2], sc[:, 0:2], AF.Exp), r=["dsc"], w=["dsc"])
                        V(lambda e: e.tensor_tensor(sc[:, 2:3], sc[:, 1:2], sc[:, 0:1], ALU.subtract))
                        V(lambda e: e.tensor_scalar(sc[:, 2:3], sc[:, 2:3], -lam_init, None, ALU.add))
                        V(lambda e: e.tensor_scalar(gs[:], gs[:], 1.0 - lam_init, None, ALU.mult))
                        neglam = sc[:, 2:3]
                        qh = Ring(nc, ph, "dqh", [128, S_], BF16, 2)
                        kh = Ring(nc, ph, "dkh", [128, S_], BF16, 2)
                        vh = Ring(nc, ph, "dvh", [128, NT, 128], BF16, 2)
                        yst = Ring(nc, ph, "dyst", [128, S_], BF16, 2)
                        p_st = Ring(nc, ph, "pDs", [128, 512], F32, 3, psum=True)
                        p_o = [ps("pDo%d" % m, [128, 512], F32, ph) for m in range(2)]
                        p_d = [ps("pDd%d" % m, [128, 512], F32, ph) for m in range(2)]
                        p_ss = ps("pDss", [128, 512], F32, ph)
                        PT = Ring(nc, ph, "dPT", [128, 512], BF16, 4)
                        tmpb = Ring(nc, ph, "dtb", [128, 128], F32, 3)
                        r1 = Ring(nc, ph, "dr1", [128, 512], F32, 2)
                        o1 = Ring(nc, ph, "do1", [128, 512], F32, 2)
                        o2 = Ring(nc, ph, "do2", [128, 512], F32, 2)
                        sqd = Ring(nc, ph, "dsq", [128, 512], BF16, 2)
                        dvv = dv.rearrange("(i p) e -> p i e", p=128)
                        for h in range(8):
                            q_, qn = qh.next()
                            k_, kn = kh.next()
                            v_, vn = vh.next()
                            ys, ysn = yst.next()
                            S.dma("sp", q_[:], fmT[16 + h], qn, w=[qn])
                            S.dma("sp", k_[:], fmT[24 + h], kn, w=[kn])
                            S.dma("sp", v_[:], dvv[:, :, h * 128:(h + 1) * 128], vn, w=[vn])
                            cb_ap = tb[:, 31 * 8 + h:31 * 8 + h + 1]
                            for cq in range(NG):
                                njb = 4 * (cq + 1)
                                for j in range(njb):
                                    jj = j - 4 * cq
                                    ii0 = max(0, jj)
                                    c0 = ii0 * 128
                                    for m in range(2):
                                        ms = slice(64 * m, 64 * m + 64)
                                        pst, pstn = p_st.next()
                                        S.op("pe", lambda e: e.matmul(pst[:, c0:512], k_[ms, j * 128:(j + 1) * 128], q_[ms, cq * 512 + c0:(cq + 1) * 512],
                                                                       start=True, stop=True), r=[kn, qn], w=[pstn])
                                        pt_, ptn = PT.next()
                                        for ii in range(ii0, 4):
                                            d = 4 * cq + ii - j
                                            if d >= 2:
                                                break
                                            tb_, tbn = tmpb.next()
                                            isl = slice(ii * 128, (ii + 1) * 128)
                                            S.op("dve", lambda e: e.scalar_tensor_tensor(tb_[:], pst[:, isl], scale, Bm[:, h, d, :], ALU.mult, ALU.add),
                                                 r=[pstn, "Bm%d_%d" % (h, d)], w=[tbn])
                                            S.op("act", lambda e: e.activation(pt_[:, isl], tb_[:], AF.Exp), r=[tbn], w=[ptn])
                                        iic = max(ii0, j + 2 - 4 * cq)
                                        if iic < 4:
                                            S.op("act", lambda e: e.activation(pt_[:, iic * 128:512], pst[:, iic * 128:512], AF.Exp, bias=cb_ap, scale=scale),
                                                 r=[pstn, "tb"], w=[ptn])
                                        S.op("pe", lambda e: e.matmul(p_o[m][:, c0:512], v_[:, j, :], pt_[:, c0:512], start=(j == 0), stop=(j == njb - 1)),
                                             r=[vn, ptn], w=["pDo%d" % m])
                                        S.op("pe", lambda e: e.matmul(p_d[m][:, c0:512], ones[:], pt_[:, c0:512], start=(j == 0), stop=(j == njb - 1)),
                                             r=["ones", ptn], w=["pDd%d" % m])
                                ts = slice(cq * 512, (cq + 1) * 512)
                                ra, ran = r1.next()
                                oa, oan = o1.next()
                                ob, obn = o2.next()
                                S.op("dve", lambda e: e.reciprocal(ra[:], p_d[0][:]), r=["pDd0"], w=[ran])
                                S.op("dve", lambda e: e.tensor_tensor(oa[:], p_o[0][:], ra[:], ALU.mult), r=["pDo0", ran], w=[oan])
                                S.op("dve", lambda e: e.reciprocal(ra[:], p_d[1][:]), r=["pDd1", ran], w=[ran])
                                S.op("dve", lambda e: e.tensor_tensor(ob[:], p_o[1][:], ra[:], ALU.mult), r=["pDo1", ran], w=[obn])
                                S.op("dve", lambda e: e.scalar_tensor_tensor(oa[:], ob[:], neglam, oa[:], ALU.mult, ALU.add), r=[obn, oan, "dsc"], w=[oan])
                                sq_, sqn = sqd.next()
                                S.op("act", lambda e: e.activation(sq_[:], oa[:], AF.Square), r=[oan], w=[sqn])
                                S.op("pe", lambda e: e.matmul(p_ss[:], ones[:], sq_[:], start=True, stop=True), r=["ones", sqn], w=["pDss"])
                                S.op("act", lambda e: e.activation(ra[:], p_ss[:], AF.Sqrt, bias=eps_ap, scale=1.0 / 128), r=["pDss", "cvec", ran], w=[ran])
                                S.op("dve", lambda e: e.reciprocal(ra[:], ra[:]), r=[ran], w=[ran])
                                S.op("dve", lambda e: e.scalar_tensor_tensor(ys[:, ts], oa[:], gs[:, 0:1], ra[:], ALU.mult, ALU.mult), r=[oan, ran, "dsc"], w=[ysn])
                            S.dma("pool", yT[8 + h], ys[:], ysn, r=[ysn])
                        S.barrier()

                if on("E"):
                    with ExitStack() as ph:
                        wo = sb("woE", [128, DC, D], BF16, ph)
                        for b in range(4):
                            S.dma("sp", wo[:, :, b * 512:(b + 1) * 512], Wb["out", l][b], "woE", r=["w_out%d" % l], w=["woE"])
                        gpost = load_gamma(ph, "gpostE", A["g_mix_post"][l])
                        yg = Ring(nc, ph, "ygE", [128, DC, 512], BF16, 2)
                        banks = Ring(nc, ph, "pE", [128, 512], F32, 8, psum=True)
                        tail = Tail(ph, "tE")
                        for tg in range(NG):
                            y_, yn = yg.next()
                            S.dma("sp", y_[:], yT[:, :, tg * 512:(tg + 1) * 512].rearrange("c p t -> p c t"), yn, w=[yn])
                            for ii in range(4):
                                i = tg * 4 + ii
                                bk = [banks.next() for _ in range(4)]
                                for cb in range(4):
                                    for c in range(DC):
                                        S.op("pe", lambda e: e.matmul(bk[cb][0][:], y_[:, c, ii * 128:(ii + 1) * 128], wo[:, c, cb * 512:(cb + 1) * 512],
                                                                       start=(c == 0), stop=(c == DC - 1)), r=[yn, "woE"], w=[bk[cb][1]])
                                tail.run([b_[0][:] for b_ in bk], [b_[1] for b_ in bk], "gpostE", gpost,
                                         xsrc[i * 128:(i + 1) * 128, :], out[q][i * 128:(i + 1) * 128, :])
                        S.barrier()

                if on("F"):
                    with ExitStack() as ph:
                        wq = sb("wqF", [128, DC, 512], BF16, ph)
                        wo = sb("woF", [128, 4, D], BF16, ph)
                        S.dma("sp", wq[:], Wb["xq", l], "wqF", r=["w_x%d" % l], w=["wqF"])
                        S.dma("sp", wo[:], Wb["xo", l], "woF", r=["w_x%d" % l], w=["woF"])
                        kxT = sb("kxT", [128, 4, MEM], BF16, ph)
                        vx = sb("vx", [128, MT, 512], BF16, ph)
                        gpre = load_gamma(ph, "gpreF", A["g_x_pre"][l])
                        gpost = load_gamma(ph, "gpostF", A["g_x_post"][l])
                        with ExitStack() as p1:
                            gm = load_gamma(p1, "gmem", A["g_mem"][l])
                            wkv = sb("wkvF", [128, DC, 1024], BF16, p1)
                            for b in range(2):
                                S.dma("sp", wkv[:, :, b * 512:(b + 1) * 512], Wb["xkv", l][b], "wkvF", r=["w_x%d" % l], w=["wkvF"])
                            memT = sb("memT", [128, DC, MEM], BF16, p1)
                            ntm = NormT(p1, "nFm")
                            for mt in range(MT):
                                ntm.run(A["mem"][q][mt * 128:(mt + 1) * 128, :], "gmem", gm, memT, "memT", mt * 128)
                            pk = Ring(nc, p1, "pFk", [128, 512], F32, 2, psum=True)
                            for h in range(4):
                                p_, pn = pk.next()
                                for c in range(DC):
                                    S.op("pe", lambda e: e.matmul(p_[:, 0:MEM], wkv[:, c, h * 128:(h + 1) * 128], memT[:, c, :], start=(c == 0), stop=(c == DC - 1)),
                                         r=["wkvF", "memT"], w=[pn])
                                S.op("act", lambda e: e.copy(kxT[:, h, :], p_[:, 0:MEM]), r=[pn], w=["kxT"])
                            for mt in range(MT):
                                p_, pn = pk.next()
                                for c in range(DC):
                                    S.op("pe", lambda e: e.matmul(p_[:], memT[:, c, mt * 128:(mt + 1) * 128], wkv[:, c, 512:1024], start=(c == 0), stop=(c == DC - 1)),
                                         r=["wkvF", "memT"], w=[pn])
                                S.op("dve", lambda e: e.tensor_copy(vx[:, mt, :], p_[:]), r=[pn], w=["vx"])
                            S.barrier()
                        hTg = sb("hTgF", [128, DC, 512], BF16, ph)
                        nt = NormT(ph, "nF")
                        oxT = sb("oxT", [128, 4, 512], BF16, ph)
                        qxr = Ring(nc, ph, "qxF", [128, 512], BF16, 2)
                        PT = Ring(nc, ph, "PTF", [128, 512], BF16, 4)
                        rr = Ring(nc, ph, "rrF", [128, 512], F32, 2)
                        pf = Ring(nc, ph, "pF", [128, 512], F32, 6, psum=True)
                        tail = Tail(ph, "tF")
                        xs_scale = 128 ** -0.5
                        for tg in range(NG):
                            for ii in range(4):
                                i = tg * 4 + ii
                                nt.run(out[q][i * 128:(i + 1) * 128, :], "gpreF", gpre, hTg, "hTgF", ii * 128)
                            for h in range(4):
                                pq, pqn = pf.next()
                                for c in range(DC):
                                    S.op("pe", lambda e: e.matmul(pq[:], wq[:, c, h * 128:(h + 1) * 128], hTg[:, c, :], start=(c == 0), stop=(c == DC - 1)),
                                         r=["wqF", "hTgF"], w=[pqn])
                                qx_, qxn = qxr.next()
                                S.op("act", lambda e: e.copy(qx_[:], pq[:]), r=[pqn], w=[qxn])
                                pts = []
                                for mt in range(MT):
                                    pst, pstn = pf.next()
                                    S.op("pe", lambda e: e.matmul(pst[:], kxT[:, h, mt * 128:(mt + 1) * 128], qx_[:], start=True, stop=True), r=["kxT", qxn], w=[pstn])
                                    pt_, ptn = PT.next()
                                    S.op("act", lambda e: e.activation(pt_[:], pst[:], AF.Exp, scale=xs_scale), r=[pstn], w=[ptn])
                                    pts.append((pt_, ptn))
                                po, pon = pf.next()
                                pd, pdn = pf.next()
                                for mt in range(MT):
                                    S.op("pe", lambda e: e.matmul(po[:], vx[:, mt, h * 128:(h + 1) * 128], pts[mt][0][:], start=(mt == 0), stop=(mt == MT - 1)),
                                         r=["vx", pts[mt][1]], w=[pon])
                                for mt in range(MT):
                                    S.op("pe", lambda e: e.matmul(pd[:], ones[:], pts[mt][0][:], start=(mt == 0), stop=(mt == MT - 1)),
                                         r=["ones", pts[mt][1]], w=[pdn])
                                r_, rn = rr.next()
                                S.op("dve", lambda e: e.reciprocal(r_[:], pd[:]), r=[pdn], w=[rn])
                                S.op("dve", lambda e: e.tensor_tensor(oxT[:, h, :], po[:], r_[:], ALU.mult), r=[pon, rn], w=["oxT%d" % h])
                            for ii in range(4):
                                i = tg * 4 + ii
                                bk = [pf.next() for _ in range(4)]
                                for cb in range(4):
                                    for hc in range(4):
                                        S.op("pe", lambda e: e.matmul(bk[cb][0][:], oxT[:, hc, ii * 128:(ii + 1) * 128], wo[:, hc, cb * 512:(cb + 1) * 512],
                                                                       start=(hc == 0), stop=(hc == 3)), r=["oxT%d" % hc, "woF"], w=[bk[cb][1]])
                                tail.run([b_[0][:] for b_ in bk], [b_[1] for b_ in bk], "gpostF", gpost,
                                         out[q][i * 128:(i + 1) * 128, :], out[q][i * 128:(i + 1) * 128, :])
                        S.barrier()

                if on("G"):
                    with ExitStack() as ph:
                        gt = sb("gG", [128, D], F32, ph)
                        GT, TPG = 512, 4
                        hTg = sb("hTgG", [128, DC, GT], BF16, ph)
                        aT = sb("aT", [128, 64, GT], BF16, ph)
                        raw = [sb("rawG%d" % i, [128, D], F32, ph) for i in range(TPG)]
                        nt = NormT(ph, "nG")
                        wu = Ring(nc, ph, "wuG", [128, DC, 256], BF16, 3)
                        wd = Ring(nc, ph, "wdG", [128, 4, 512], BF16, 4)
                        pup = Ring(nc, ph, "pGu", [128, 512], F32, 2, psum=True)
                        pdn = [ps("pGd%d" % i, [128, 512], F32, ph) for i in range(TPG)]
                        rl = Ring(nc, ph, "rlG", [128, GT], F32, 2)
                        ssq = sb("ssqG", [128, 32], F32, ph)
                        junk = sb("junkG", [128, 512], BF16, ph)
                        for tg in range(S_ // GT):
                            S.dma("sp", gt[:], A["g_mlp_pre"][l].partition_broadcast(128), "gG", w=["gG"])
                            for ii in range(TPG):
                                i = tg * TPG + ii
                                nt.run(out[q][i * 128:(i + 1) * 128, :], "gG", gt, hTg, "hTgG", ii * 128)
                            S.dma("sp", gt[:], A["g_mlp_post"][l].partition_broadcast(128), "gG", w=["gG"])
                            for b in range(32 if cfg.gcut >= 2 else 0):
                                w_, wn = wu.next()
                                S.dma("sp", w_[:], Wb["up", l][b], wn, r=["w_up%d" % l], w=[wn])
                                for f2 in range(2):
                                    f = b * 2 + f2
                                    p_, pn = pup.next()
                                    for c in range(DC):
                                        S.op("pe", lambda e: e.matmul(p_[:, 0:GT], w_[:, c, f2 * 128:(f2 + 1) * 128], hTg[:, c, :], start=(c == 0), stop=(c == DC - 1)),
                                             r=["hTgG", wn], w=[pn])
                                    r_, rn = rl.next()
                                    S.op("dve", lambda e: e.tensor_scalar(r_[:], p_[:, 0:GT], 0.0, None, ALU.max), r=[pn], w=[rn])
                                    S.op("act", lambda e: e.activation(aT[:, f, :], r_[:], AF.Square), r=[rn], w=["aT%d" % f])
                            for cq4 in range(4 if cfg.gcut >= 3 else 0):
                                for f4 in range(16):
                                    w_, wn = wd.next()
                                    S.dma("sp", w_[:], Wb["down", l][f4 * 512:(f4 + 1) * 512, cq4 * 512:(cq4 + 1) * 512].rearrange("(f p) n -> p f n", p=128),
                                          wn, r=["w_down%d" % l], w=[wn])
                                    for fi in range(4):
                                        f = f4 * 4 + fi
                                        for ii in range(TPG):
                                            S.op("pe", lambda e: e.matmul(pdn[ii][:], aT[:, f, ii * 128:(ii + 1) * 128], w_[:, fi, :], start=(f == 0), stop=(f == 63)),
                                                 r=["aT%d" % f, wn], w=["pGd%d" % ii])
                                for ii in range(TPG if cfg.gcut >= 4 else 0):
                                    S.op("act", lambda e: e.activation(junk[:], pdn[ii][:], AF.Square, accum_out=ssq[:, ii * 8 + cq4:ii * 8 + cq4 + 1]),
                                         r=["pGd%d" % ii], w=["junkG", "ssqG%d" % ii, "pGdx%d" % ii])
                                    S.op("dve", lambda e: e.tensor_copy(raw[ii][:, cq4 * 512:(cq4 + 1) * 512], pdn[ii][:]),
                                         r=["pGd%d" % ii, "pGdx%d" % ii], w=["rawG%d" % ii])
                            for ii in range(TPG if cfg.gcut >= 5 else 0):
                                i = tg * TPG + ii
                                sn = "ssqG%d" % ii
                                xs, xn = nt.xin.next()
                                S.dma("sp", xs[:], out[q][i * 128:(i + 1) * 128, :], xn, w=[xn])
                                S.op("dve", lambda e: e.tensor_reduce(ssq[:, ii * 8 + 4:ii * 8 + 5], ssq[:, ii * 8:ii * 8 + 4], AX.X, ALU.add), r=[sn], w=[sn])
                                S.op("act", lambda e: e.activation(ssq[:, ii * 8 + 5:ii * 8 + 6], ssq[:, ii * 8 + 4:ii * 8 + 5], AF.Sqrt, bias=eps_ap, scale=1.0 / D), r=[sn, "cvec"], w=[sn])
                                S.op("dve", lambda e: e.reciprocal(ssq[:, ii * 8 + 5:ii * 8 + 6], ssq[:, ii * 8 + 5:ii * 8 + 6]), r=[sn], w=[sn])
                                S.op("dve", lambda e: e.tensor_scalar(raw[ii][:], raw[ii][:], ssq[:, ii * 8 + 5:ii * 8 + 6], None, ALU.mult),
                                     r=[sn, "rawG%d" % ii], w=["rawG%d" % ii])
                                S.op("dve", lambda e: e.tensor_tensor(raw[ii][:], raw[ii][:], gt[:], ALU.mult),
                                     r=["gG", "rawG%d" % ii], w=["rawG%d" % ii])
                                S.op("pool", lambda e: e.tensor_tensor(raw[ii][:], raw[ii][:], xs[:], ALU.add), r=[xn, "rawG%d" % ii], w=["rawG%d" % ii])
                                S.dma("pool", out[q][i * 128:(i + 1) * 128, :], raw[ii][:], "rawG%d" % ii, r=["rawG%d" % ii])
                        S.barrier()

        S.barrier()
    return nc


def kernel(**inputs):
    n_cores = 8
    x = np.ascontiguousarray(np.asarray(inputs["x"], dtype=np.float32))
    B, S_, _ = x.shape
    nseq = B // n_cores
    mem = np.ascontiguousarray(np.asarray(inputs["mem"], dtype=np.float32))
    pos = np.ascontiguousarray(np.asarray(inputs["positions"]).astype(np.int32))
    depth = int(np.asarray(inputs["w_in"]).shape[0])
    cfg = Cfg(S=S_, NSEQ=nseq, DEPTH=depth, MEM=mem.shape[1])
    nc = build(cfg)
    consts = host_consts()
    shared = {}
    for name, _ in PARAM_SPECS:
        shared[name] = np.ascontiguousarray(np.asarray(inputs[name], dtype=np.float32))
    for k, v in consts.items():
        shared["c_" + k] = v
    in_maps = []
    for c in range(n_cores):
        m = dict(shared)
        m["x"] = x[c * nseq:(c + 1) * nseq]
        m["mem"] = mem[c * nseq:(c + 1) * nseq]
        m["positions"] = pos[c * nseq:(c + 1) * nseq]
        in_maps.append(m)
    res = run_bass_kernel_spmd(nc, in_maps, core_ids=list(range(n_cores)))
    return np.concatenate([np.asarray(r["out"], dtype=np.float32) for r in res.results], axis=0)
```
